# Optimizing a Trainium2 kernel written in Bass

```python
import math
import jax, jax.numpy as jnp
from jax import lax
import numpy as np

D_MODEL = 1024
BATCH = 8
SEQ = 4096
DEPTH = 2

MLA_HEADS = 4
QK_NOPE_DIM = 128
QK_ROPE_DIM = 64
V_HEAD_DIM = 128
Q_LORA_RANK = 384
KV_LORA_RANK = 256
CONV_CHANNELS = D_MODEL - MLA_HEADS * V_HEAD_DIM
CONV_WIDTH = 31
MLA_IN_DIM = Q_LORA_RANK + KV_LORA_RANK + QK_ROPE_DIM + 2 * CONV_CHANNELS
DIFF_HEAD_DIM = 128
DIFF_HEADS = D_MODEL // (2 * DIFF_HEAD_DIM)
DIFF_QKV_DIM = 3 * DIFF_HEADS * 2 * DIFF_HEAD_DIM
N_EXPERTS = 32
TOP_K = 4
D_EXPERT = D_MODEL
SWIGLU_LIMIT = 7.0
SWIGLU_ALPHA = 1.702
EXPERT_BLOCK = 256
ROPE_THETA = 10000.0
Q_BLOCK = 128
DN_ALPHA = (2 * DEPTH) ** 0.25
DN_BETA = (8 * DEPTH) ** -0.25
LN_EPS = 1e-5
RMS_EPS = 1e-6

kernel_name = 'hybrid_mla_conformer_diffattn_moe'


def layer_norm(x, g, b):
    xf = x.astype(jnp.float32)
    mu = jnp.mean(xf, -1, keepdims=True)
    var = jnp.mean(jnp.square(xf - mu), -1, keepdims=True)
    y = (xf - mu) * lax.rsqrt(var + LN_EPS)
    return (y * g.astype(jnp.float32) + b.astype(jnp.float32)).astype(x.dtype)


def rms_norm(x, g):
    xf = x.astype(jnp.float32)
    y = xf * lax.rsqrt(jnp.mean(jnp.square(xf), -1, keepdims=True) + RMS_EPS)
    return (y * g.astype(jnp.float32)).astype(x.dtype)


def apply_rope(x):
    seq, dim = x.shape[1], x.shape[-1]
    inv_freq = 1.0 / (ROPE_THETA ** (jnp.arange(0, dim, 2, dtype=jnp.float32) / dim))
    ang = jnp.arange(seq, dtype=jnp.float32)[:, None] * inv_freq[None, :]
    cos = jnp.cos(ang)[:, None, :]
    sin = jnp.sin(ang)[:, None, :]
    xf = x.astype(jnp.float32)
    x1, x2 = xf[..., : dim // 2], xf[..., dim // 2:]
    return jnp.concatenate([x1 * cos - x2 * sin, x2 * cos + x1 * sin], -1).astype(x.dtype)


def causal_multi_map_attention(q, k, v, coef, scale):
    b, h, m, s, dk = q.shape
    nb = s // Q_BLOCK
    q_blocks = jnp.moveaxis(q.reshape(b, h, m, nb, Q_BLOCK, dk), 3, 0)
    k_pos = jnp.arange(s)

    def one_block(args):
        q_blk, blk = args
        scores = jnp.einsum('bhmqd,bhmkd->bhmqk', q_blk, k).astype(jnp.float32) * scale
        q_pos = blk * Q_BLOCK + jnp.arange(Q_BLOCK)
        causal = k_pos[None, :] <= q_pos[:, None]
        probs = jax.nn.softmax(jnp.where(causal, scores, -1e30), axis=-1)
        weights = jnp.einsum('bhmqk,m->bhqk', probs, coef)
        return jnp.einsum('bhqk,bhkd->bhqd', weights.astype(v.dtype), v)

    out = lax.map(one_block, (q_blocks, jnp.arange(nb)))
    return jnp.moveaxis(out, 0, 2).reshape(b, h, s, v.shape[-1])


def mla_conv_mixer(h, w_in, q_norm_g, w_q_up, kv_norm_g, w_kv_up, conv_w, conv_b,
                   conv_ln_g, conv_ln_b, w_o):
    b, s, _ = h.shape
    proj = h @ w_in
    o1 = Q_LORA_RANK
    o2 = o1 + KV_LORA_RANK
    o3 = o2 + QK_ROPE_DIM
    q_lat, kv_lat, k_rope, conv_in = proj[..., :o1], proj[..., o1:o2], proj[..., o2:o3], proj[..., o3:]
    q = (rms_norm(q_lat, q_norm_g) @ w_q_up).reshape(b, s, MLA_HEADS, QK_NOPE_DIM + QK_ROPE_DIM)
    q = jnp.concatenate([q[..., :QK_NOPE_DIM], apply_rope(q[..., QK_NOPE_DIM:])], -1)
    kv = (rms_norm(kv_lat, kv_norm_g) @ w_kv_up).reshape(b, s, MLA_HEADS, QK_NOPE_DIM + V_HEAD_DIM)
    k_nope, v = kv[..., :QK_NOPE_DIM], kv[..., QK_NOPE_DIM:]
    k_rope = jnp.broadcast_to(apply_rope(k_rope[:, :, None, :]), (b, s, MLA_HEADS, QK_ROPE_DIM))
    k = jnp.concatenate([k_nope, k_rope], -1)
    attn = causal_multi_map_attention(
        q.transpose(0, 2, 1, 3)[:, :, None], k.transpose(0, 2, 1, 3)[:, :, None],
        v.transpose(0, 2, 1, 3), jnp.ones((1,), jnp.float32),
        (QK_NOPE_DIM + QK_ROPE_DIM) ** -0.5)
    attn = attn.transpose(0, 2, 1, 3).reshape(b, s, MLA_HEADS * V_HEAD_DIM)
    a, gate = conv_in[..., :CONV_CHANNELS], conv_in[..., CONV_CHANNELS:]
    u = a * jax.nn.sigmoid(gate)
    u = lax.conv_general_dilated(
        u, conv_w[:, None, :], window_strides=(1,), padding=[(CONV_WIDTH - 1, 0)],
        dimension_numbers=('NWC', 'WIO', 'NWC'), feature_group_count=CONV_CHANNELS) + conv_b
    u = jax.nn.silu(layer_norm(u, conv_ln_g, conv_ln_b))
    return jnp.concatenate([attn, u], -1) @ w_o


def diff_attn_mixer(h, w_qkv, lambda_q1, lambda_k1, lambda_q2, lambda_k2, subln_g, w_o, lambda_init):
    b, s, _ = h.shape
    qk_w = DIFF_HEADS * 2 * DIFF_HEAD_DIM
    proj = h @ w_qkv
    q = apply_rope(proj[..., :qk_w].reshape(b, s, 2 * DIFF_HEADS, DIFF_HEAD_DIM))
    k = apply_rope(proj[..., qk_w:2 * qk_w].reshape(b, s, 2 * DIFF_HEADS, DIFF_HEAD_DIM))
    v = proj[..., 2 * qk_w:].reshape(b, s, DIFF_HEADS, 2 * DIFF_HEAD_DIM)
    q = q.reshape(b, s, DIFF_HEADS, 2, DIFF_HEAD_DIM).transpose(0, 2, 3, 1, 4)
    k = k.reshape(b, s, DIFF_HEADS, 2, DIFF_HEAD_DIM).transpose(0, 2, 3, 1, 4)
    f32 = jnp.float32
    lam = (jnp.exp(jnp.sum(lambda_q1.astype(f32) * lambda_k1.astype(f32)))
           - jnp.exp(jnp.sum(lambda_q2.astype(f32) * lambda_k2.astype(f32))) + lambda_init)
    coef = jnp.stack([jnp.ones_like(lam), -lam])
    attn = causal_multi_map_attention(q, k, v.transpose(0, 2, 1, 3), coef, DIFF_HEAD_DIM ** -0.5)
    attn = rms_norm(attn, subln_g) * (1.0 - lambda_init)
    return attn.transpose(0, 2, 1, 3).reshape(b, s, DIFF_HEADS * 2 * DIFF_HEAD_DIM) @ w_o


def moe_ffn(h, router_w, router_b, w_gu, b_gu, w_dn, b_dn):
    b, s, d = h.shape
    t = b * s
    xf = h.reshape(t, d)
    logits = (xf @ router_w + router_b).astype(jnp.float32)
    top_val, top_idx = lax.top_k(logits, TOP_K)
    gates = jax.nn.softmax(top_val, axis=-1)
    n_assign = t * TOP_K
    flat_e = top_idx.reshape(-1)
    flat_g = gates.reshape(-1)
    flat_tok = jnp.arange(n_assign, dtype=jnp.int32) // TOP_K
    order = jnp.argsort(flat_e)
    se, stok, sg = flat_e[order], flat_tok[order], flat_g[order]
    counts = jnp.bincount(flat_e, length=N_EXPERTS)
    padded = ((counts + EXPERT_BLOCK - 1) // EXPERT_BLOCK) * EXPERT_BLOCK
    pend = jnp.cumsum(padded)
    pstart = pend - padded
    ustart = jnp.cumsum(counts) - counts
    dest = pstart[se] + jnp.arange(n_assign, dtype=jnp.int32) - ustart[se]
    n_rows = ((n_assign + EXPERT_BLOCK - 1) // EXPERT_BLOCK) * EXPERT_BLOCK + N_EXPERTS * EXPERT_BLOCK
    n_blk = n_rows // EXPERT_BLOCK
    row_tok = jnp.zeros((n_rows,), jnp.int32).at[dest].set(stok)
    row_gate = jnp.zeros((n_rows,), jnp.float32).at[dest].set(sg)
    blk_start = jnp.arange(n_blk, dtype=jnp.int32) * EXPERT_BLOCK
    blk_e = jnp.minimum(jnp.searchsorted(pend, blk_start, side='right'), N_EXPERTS - 1)

    def expert_block(args):
        tok, g, e = args
        hgu = xf[tok] @ w_gu[e] + b_gu[e]
        gate = jnp.minimum(hgu[:, :D_EXPERT], SWIGLU_LIMIT)
        up = jnp.clip(hgu[:, D_EXPERT:], -SWIGLU_LIMIT, SWIGLU_LIMIT)
        act = (up + 1.0) * gate * jax.nn.sigmoid(SWIGLU_ALPHA * gate)
        y = act @ w_dn[e] + b_dn[e]
        return y * g[:, None].astype(y.dtype)

    ys = lax.map(expert_block, (row_tok.reshape(n_blk, EXPERT_BLOCK),
                                row_gate.reshape(n_blk, EXPERT_BLOCK), blk_e))
    out = jnp.zeros((t, d), h.dtype).at[row_tok].add(ys.reshape(n_rows, d).astype(h.dtype))
    return out.reshape(b, s, d)


def setup_inputs(seed: int = 0) -> dict:
    key = jax.random.key(seed)
    keys = iter(jax.random.split(key, 64))

    def nrm(shape, scale):
        return jax.random.normal(next(keys), shape, jnp.float32) * scale

    def gain(n):
        return 1.0 + nrm((n,), 0.02)

    def bias(shape):
        return nrm(shape, 0.01)

    def add_moe(p, pre):
        p[pre + 'router_w'] = nrm((D_MODEL, N_EXPERTS), D_MODEL ** -0.5)
        p[pre + 'router_b'] = bias((N_EXPERTS,))
        p[pre + 'w_gu'] = nrm((N_EXPERTS, D_MODEL, 2 * D_EXPERT), D_MODEL ** -0.5)
        p[pre + 'b_gu'] = bias((N_EXPERTS, 2 * D_EXPERT))
        p[pre + 'w_dn'] = nrm((N_EXPERTS, D_EXPERT, D_MODEL), DN_BETA * D_EXPERT ** -0.5)
        p[pre + 'b_dn'] = bias((N_EXPERTS, D_MODEL))

    p = {'x': nrm((BATCH, SEQ, D_MODEL), 1.0)}
    p['l0_w_in'] = nrm((D_MODEL, MLA_IN_DIM), D_MODEL ** -0.5)
    p['l0_q_norm_g'] = gain(Q_LORA_RANK)
    p['l0_w_q_up'] = nrm((Q_LORA_RANK, MLA_HEADS * (QK_NOPE_DIM + QK_ROPE_DIM)), Q_LORA_RANK ** -0.5)
    p['l0_kv_norm_g'] = gain(KV_LORA_RANK)
    p['l0_w_kv_up'] = nrm((KV_LORA_RANK, MLA_HEADS * (QK_NOPE_DIM + V_HEAD_DIM)), KV_LORA_RANK ** -0.5)
    p['l0_conv_w'] = nrm((CONV_WIDTH, CONV_CHANNELS), CONV_WIDTH ** -0.5)
    p['l0_conv_b'] = bias((CONV_CHANNELS,))
    p['l0_conv_ln_g'] = gain(CONV_CHANNELS)
    p['l0_conv_ln_b'] = bias((CONV_CHANNELS,))
    p['l0_w_o'] = nrm((MLA_HEADS * V_HEAD_DIM + CONV_CHANNELS, D_MODEL), DN_BETA * D_MODEL ** -0.5)
    p['l0_ln1_g'] = gain(D_MODEL)
    p['l0_ln1_b'] = bias((D_MODEL,))
    add_moe(p, 'l0_')
    p['l0_ln2_g'] = gain(D_MODEL)
    p['l0_ln2_b'] = bias((D_MODEL,))
    p['l1_w_qkv'] = nrm((D_MODEL, DIFF_QKV_DIM), D_MODEL ** -0.5)
    p['l1_lambda_q1'] = nrm((DIFF_HEAD_DIM,), 0.1)
    p['l1_lambda_k1'] = nrm((DIFF_HEAD_DIM,), 0.1)
    p['l1_lambda_q2'] = nrm((DIFF_HEAD_DIM,), 0.1)
    p['l1_lambda_k2'] = nrm((DIFF_HEAD_DIM,), 0.1)
    p['l1_subln_g'] = gain(2 * DIFF_HEAD_DIM)
    p['l1_w_o'] = nrm((DIFF_HEADS * 2 * DIFF_HEAD_DIM, D_MODEL), DN_BETA * D_MODEL ** -0.5)
    p['l1_ln1_g'] = gain(D_MODEL)
    p['l1_ln1_b'] = bias((D_MODEL,))
    add_moe(p, 'l1_')
    p['l1_ln2_g'] = gain(D_MODEL)
    p['l1_ln2_b'] = bias((D_MODEL,))
    return p


def reference(x,
              l0_w_in, l0_q_norm_g, l0_w_q_up, l0_kv_norm_g, l0_w_kv_up, l0_conv_w, l0_conv_b,
              l0_conv_ln_g, l0_conv_ln_b, l0_w_o, l0_ln1_g, l0_ln1_b,
              l0_router_w, l0_router_b, l0_w_gu, l0_b_gu, l0_w_dn, l0_b_dn, l0_ln2_g, l0_ln2_b,
              l1_w_qkv, l1_lambda_q1, l1_lambda_k1, l1_lambda_q2, l1_lambda_k2, l1_subln_g, l1_w_o,
              l1_ln1_g, l1_ln1_b,
              l1_router_w, l1_router_b, l1_w_gu, l1_b_gu, l1_w_dn, l1_b_dn, l1_ln2_g, l1_ln2_b):
    layer_params = (
        ((l0_w_in, l0_q_norm_g, l0_w_q_up, l0_kv_norm_g, l0_w_kv_up, l0_conv_w, l0_conv_b,
          l0_conv_ln_g, l0_conv_ln_b, l0_w_o),
         (l0_ln1_g, l0_ln1_b),
         (l0_router_w, l0_router_b, l0_w_gu, l0_b_gu, l0_w_dn, l0_b_dn),
         (l0_ln2_g, l0_ln2_b)),
        ((l1_w_qkv, l1_lambda_q1, l1_lambda_k1, l1_lambda_q2, l1_lambda_k2, l1_subln_g, l1_w_o),
         (l1_ln1_g, l1_ln1_b),
         (l1_router_w, l1_router_b, l1_w_gu, l1_b_gu, l1_w_dn, l1_b_dn),
         (l1_ln2_g, l1_ln2_b)),
    )
    for i in range(DEPTH):
        mixer_p, ln1, moe_p, ln2 = layer_params[i]
        if i % 2 == 0:
            mix = mla_conv_mixer(x, *mixer_p)
        else:
            mix = diff_attn_mixer(x, *mixer_p, lambda_init=0.8 - 0.6 * math.exp(-0.3 * i))
        x = layer_norm(DN_ALPHA * x + mix, *ln1)
        x = layer_norm(DN_ALPHA * x + moe_ffn(x, *moe_p), *ln2)
    return x
```

```python
import contextlib
import math
import numpy as np
import concourse.bass as bass
import concourse.mybir as mybir
from concourse.bass_utils import run_bass_kernel_spmd

F32 = mybir.dt.float32
BF16 = mybir.dt.bfloat16
I32 = mybir.dt.int32
U32 = mybir.dt.uint32
U8 = mybir.dt.uint8
AF = mybir.ActivationFunctionType
ALU = mybir.AluOpType
AX = mybir.AxisListType
DSZ = {F32: 4, BF16: 2, I32: 4, U32: 4, U8: 1}

COMPUTE = ("pe", "dve", "act", "pool")
ISSUERS = ("pe", "dve", "act", "pool", "sp")

S = 4096
D = 1024
NT = 32
NB = 8
E = 32
CAPS = (768, 1024)
CAPMAX = 1024
NJMAX = CAPMAX // 128
NSLOT = E * CAPMAX
DN_ALPHA = 4.0 ** 0.25
LN_EPS = 1e-5
RMS_EPS = 1e-6
LIMIT = 7.0
SW_ALPHA = 1.702


class Sem:
    def __init__(self, handle, name, step):
        self.handle, self.name, self.step, self.count = handle, name, step, 0


class Op:
    __slots__ = ("eng", "fn", "waits", "inc", "sem", "semval", "is_dma", "line")

    def __init__(self, eng, fn, sem, is_dma):
        self.eng, self.fn, self.sem, self.is_dma = eng, fn, sem, is_dma
        self.waits = []
        self.inc = is_dma
        self.semval = None
        import sys as _s
        f = _s._getframe(3)
        self.line = (f.f_lineno, f.f_back.f_lineno if f.f_back else 0)


class PseudoDep:
    def __init__(self, sem, val):
        self.sem, self.semval = sem, val


class Plan:
    def __init__(self, nc):
        self.nc = nc
        self.stack = contextlib.ExitStack()
        self.ops = {e: [] for e in ISSUERS}
        self.dma_sems = []
        self.esem = {e: self.new_sem("e_" + e, 1) for e in COMPUTE}
        self.last_w = {}
        self.readers = {}
        self.names = None

    def new_sem(self, name, step=16):
        h = self.stack.enter_context(self.nc.semaphore(name))
        s = Sem(h, name, step)
        if step == 16:
            self.dma_sems.append(s)
        return s

    def sbuf(self, name, shape, dtype):
        return self.stack.enter_context(self.nc.sbuf_tensor(name, list(shape), dtype))

    def psum(self, name, shape, dtype):
        return self.stack.enter_context(self.nc.psum_tensor(name, list(shape), dtype))

    def _add(self, op, reads, writes):
        deps = {}
        for t in reads:
            d = self.last_w.get(t)
            if d is not None:
                deps[id(d)] = (d, True)
        for t in writes:
            d = self.last_w.get(t)
            if d is not None and id(d) not in deps:
                deps[id(d)] = (d, False)
            for r in self.readers.get(t, {}).values():
                if id(r) not in deps:
                    deps[id(r)] = (r, False)
        for d, raw in deps.values():
            if d is op:
                continue
            need = True
            if (not d.is_dma) and (not op.is_dma) and d.eng == op.eng:
                need = raw and op.eng != "pe"
            if need:
                if d.is_dma:
                    op.waits.append(PseudoDep(d.sem, d.sem.count))
                else:
                    d.inc = True
                    op.waits.append(d)
        for t in writes:
            self.last_w[t] = op
            self.readers[t] = {}
        for t in reads:
            key = op.sem.name if op.is_dma else op.eng
            self.readers.setdefault(t, {})[key] = op
        self.ops[op.eng].append(op)
        return op

    def c(self, eng, fn, reads=(), writes=()):
        return self._add(Op(eng, fn, self.esem[eng], False), reads, writes)

    def dma(self, eng, fn, sem, reads=(), writes=()):
        op = Op(eng, fn, sem, True)
        op.semval = sem.count + 16
        self._add(op, reads, writes)
        sem.count += 16
        return op

    def barrier(self):
        lasts = {}
        for e in COMPUTE:
            for op in reversed(self.ops[e]):
                if not op.is_dma and op.fn is not None:
                    op.inc = True
                    lasts[e] = op
                    break
        dmas = [PseudoDep(s, s.count) for s in self.dma_sems if s.count > 0]
        for q in ISSUERS:
            w = Op(q, None, None, False)
            w.waits = list(lasts.values()) + dmas
            self.ops[q].append(w)
        self.last_w = {}
        self.readers = {}

    def emit(self):
        nc = self.nc
        for e in COMPUTE:
            cnt = 0
            for op in self.ops[e]:
                if op.fn is not None and op.inc and not op.is_dma:
                    cnt += 1
                    op.semval = cnt
        final = [(s, s.count) for s in self.dma_sems if s.count > 0]
        plan = self

        def replay(engname, eh, tail=False):
            waited = {}
            for op in plan.ops[engname]:
                for d in op.waits:
                    v = d.semval
                    assert v is not None
                    if waited.get(d.sem.name, 0) < v:
                        eh.wait_ge(d.sem.handle, v)
                        waited[d.sem.name] = v
                if op.fn is None:
                    continue
                ins = op.fn(eh)
                if plan.names is not None:
                    plan.names[ins.ins.name] = (engname, op.line)
                if op.inc:
                    ins.then_inc(op.sem.handle, op.sem.step)
            if tail:
                for s, v in final:
                    if waited.get(s.name, 0) < v:
                        eh.wait_ge(s.handle, v)

        with nc.Block() as block:
            @block.tensor
            def _(pe):
                replay("pe", pe)

            @block.vector
            def _(dve):
                replay("dve", dve)

            @block.scalar
            def _(act):
                replay("act", act)

            @block.gpsimd
            def _(pool):
                replay("pool", pool)

            @block.sync
            def _(sp):
                replay("sp", sp, tail=True)


class Arena:
    def __init__(self, p, nbytes):
        self.t = p.sbuf("arena", [128, nbytes], U8)
        self.cap = nbytes
        self.off = 0
        self.hw = 0

    def reset(self, to=0):
        self.off = to

    def take(self, shape, dtype, parts=128):
        shape = list(shape)
        n = int(np.prod(shape)) * DSZ[dtype]
        n_al = (n + 63) // 64 * 64
        assert self.off + n_al <= self.cap, ("arena overflow", self.off, n_al, self.cap)
        ap = self.t[0:parts, self.off:self.off + n].bitcast(dtype)
        self.off += n_al
        self.hw = max(self.hw, self.off)
        if len(shape) > 1:
            names = [f"d{i}" for i in range(len(shape))]
            kw = {nm: sz for nm, sz in zip(names[:-1], shape[:-1])}
            ap = ap.rearrange("p (" + " ".join(names) + ") -> p " + " ".join(names), **kw)
        return ap


def build_program(dbg=None, stop_after=None):
    dbg = dbg or set()
    nc = bass.Bass("TRN2", target_bir_lowering=False)
    p = Plan(nc)

    def din(name, shape, dt=F32):
        return nc.dram_tensor(name, list(shape), dt, kind="ExternalInput").ap()

    def dscr(name, shape, dt):
        kind = "ExternalOutput" if name in dbg else "Internal"
        return nc.dram_tensor(name, list(shape), dt, kind=kind).ap()

    x_in = din("x", [S, D])
    W = {}
    for l in (0, 1):
        pre = f"l{l}_"
        if l == 0:
            W[pre + "w_in"] = din(pre + "w_in", [D, 1728])
            W[pre + "q_norm_g"] = din(pre + "q_norm_g", [384])
            W[pre + "w_q_up"] = din(pre + "w_q_up", [384, 768])
            W[pre + "kv_norm_g"] = din(pre + "kv_norm_g", [256])
            W[pre + "w_kv_up"] = din(pre + "w_kv_up", [256, 1024])
            W[pre + "conv_w"] = din(pre + "conv_w", [31, 512])
            for n in ("conv_b", "conv_ln_g", "conv_ln_b"):
                W[pre + n] = din(pre + n, [512])
        else:
            W[pre + "w_qkv"] = din(pre + "w_qkv", [D, 3072])
            for n in ("lambda_q1", "lambda_k1", "lambda_q2", "lambda_k2"):
                W[pre + n] = din(pre + n, [128])
            W[pre + "subln_g"] = din(pre + "subln_g", [256])
        W[pre + "w_o"] = din(pre + "w_o", [D, D])
        for n in ("ln1_g", "ln1_b", "ln2_g", "ln2_b"):
            W[pre + n] = din(pre + n, [D])
        W[pre + "router_w"] = din(pre + "router_w", [D, E])
        W[pre + "router_b"] = din(pre + "router_b", [E])
        W[pre + "w_gu"] = din(pre + "w_gu", [E, D, 2 * D])
        W[pre + "b_gu"] = din(pre + "b_gu", [E, 2 * D])
        W[pre + "w_dn"] = din(pre + "w_dn", [E, D, D])
        W[pre + "b_dn"] = din(pre + "b_dn", [E, D])
    c_ident = din("c_ident", [128, 128])
    c_ltri = din("c_ltri", [128, 128])
    c_mask = din("c_mask", [128, 4, 512])
    c_cos64 = din("c_cos64", [64, S])
    c_sin64 = din("c_sin64", [64, S])
    c_cos128 = din("c_cos128", [128, S])
    c_sin128 = din("c_sin128", [128, S])
    c_iota = din("c_iota", [128, 3, 32])
    c_tokid = din("c_tokid", [128, NT, 4], I32)
    c_zero = din("c_zero", [128, E * NJMAX], I32)
    out_ap = nc.dram_tensor("out", [S, D], F32, kind="ExternalOutput").ap()

    x1f = dscr("x1f", [S, D], F32)
    x1b = dscr("x1b", [S + 128, D], BF16)
    x2f = dscr("x2f", [S, D], F32)
    Ybuf = dscr("Ybuf", [4 * S + 128, D], F32)
    tokidx = dscr("tokidx", [128 * E * NJMAX, 1], I32)
    QnT = dscr("QnT", [4, 128, S], BF16)
    QrT = dscr("QrT", [4, 64, S], BF16)
    KnT = dscr("KnT", [4, 128, S], BF16)
    KrT = dscr("KrT", [64, S], BF16)
    Vx0 = dscr("Vx0", [4, NT, 128, 129], BF16)
    convT = dscr("convT", [4, 128, S], BF16)
    QT1 = dscr("QT1", [8, 128, S], BF16)
    KT1 = dscr("KT1", [8, 128, S], BF16)
    Vx1 = dscr("Vx1", [4, NT, 128, 257], BF16)
    attnD = dscr("attnD", [8, 128, S], BF16)
    dbgD = dscr("dbgD", [128, 4096], F32)

    A = Arena(p, 204 * 1024)
    pb = [p.psum(f"pb{i}", [128, 512], F32)[:] for i in range(8)]
    pbn = [f"pb{i}" for i in range(8)]

    def MM(out, lhsT, rhs, start, stop, r, w, skip=False):
        p.c("pe", lambda e: e.matmul(out, lhsT, rhs, start=start, stop=stop, skip_group_check=skip), r, w)

    def TR(out, in_, ident, r, w):
        p.c("pe", lambda e: e.transpose(out, in_, ident), r, w)

    def TS(eng, out, in0, s1, s2, op0, op1, r, w):
        if op1 is None:
            p.c(eng, lambda e: e.tensor_scalar(out, in0, s1, None, op0), r, w)
        else:
            p.c(eng, lambda e: e.tensor_scalar(out, in0, s1, s2, op0, op1), r, w)

    def TT(eng, out, in0, in1, op, r, w):
        p.c(eng, lambda e: e.tensor_tensor(out, in0, in1, op), r, w)

    def STT(eng, out, in0, sc, in1, op0, op1, r, w):
        p.c(eng, lambda e: e.scalar_tensor_tensor(out, in0, sc, in1, op0, op1), r, w)

    def ACT(out, in_, func, r, w, bias=None, scale=1.0, accum=None):
        def fn(e):
            kw = {}
            if bias is not None:
                kw["bias"] = bias
            if accum is not None:
                kw["accum_out"] = accum
            return e.activation(out, in_, func, scale=scale, **kw)
        p.c("act", fn, r, w)

    def RSQRT(out, in_, scale, eps, r, w):
        ACT(out, in_, AF.Sqrt, r, w, bias=epsT[:, 0:1] if eps == LN_EPS else epsT[:, 1:2], scale=scale)
        p.c("dve", lambda e: e.reciprocal(out, out), w, w)

    def CP(eng, out, in_, r, w):
        if eng == "act":
            p.c("act", lambda e: e.copy(out, in_), r, w)
        else:
            p.c(eng, lambda e: e.tensor_copy(out, in_), r, w)

    def MS(eng, out, val, w):
        p.c(eng, lambda e: e.memset(out, val), (), w)

    def DMA(q, out, in_, sem, r, w, slow=False):
        if q == "pool":
            sem = SEM(sem.name[2:] + "_sw")
        if slow:
            p.dma(q, lambda e: e.dma_start(out=out, in_=in_, allow_slow_non_contiguous=True), sem, r, w)
        else:
            p.dma(q, lambda e: e.dma_start(out=out, in_=in_), sem, r, w)

    def GATHER(out, src, idx, sem, r, w):
        nrows = src.shape[0]
        p.dma("pool", lambda e: e.indirect_dma_start(
            out=out, out_offset=None, in_=src,
            in_offset=bass.IndirectOffsetOnAxis(ap=idx, axis=0)), sem, r, w)

    def SCATTER(dst, src, idx, sem, r, w):
        nrows = dst.shape[0]
        p.dma("pool", lambda e: e.indirect_dma_start(
            out=dst, out_offset=bass.IndirectOffsetOnAxis(ap=idx, axis=0),
            in_=src, in_offset=None), sem, r, w)

    sem_pool = {}

    def SEM(name):
        if name.startswith("d_"):
            name = name[2:]
        if name not in sem_pool:
            sem_pool[name] = p.new_sem("d_" + name)
        return sem_pool[name]

    bank_rr = [0]

    def nbank(lo=0, hi=7):
        b = lo + bank_rr[0] % (hi - lo)
        bank_rr[0] += 1
        return b

    def colvec(dst, src_vec, n, sem, wtok):
        DMA("sp", dst, src_vec.rearrange("(c p) -> p c", p=128), sem, (), [wtok], slow=True)

    identF = A.take([128], F32)
    identB = A.take([128], BF16)
    onesF = A.take([128], F32)
    onesB = A.take([128], BF16)
    ltriB = A.take([128], BF16)
    maskB = A.take([4, 512], BF16)
    iotaF = A.take([3, 32], F32)
    tokid = A.take([NT, 4], I32)
    s_c = SEM("const")
    DMA("sp", identF, c_ident, s_c, (), ["identF"])
    DMA("pool", identB, c_ident, s_c, (), ["identB"])
    DMA("pool", ltriB, c_ltri, s_c, (), ["ltriB"])
    DMA("pool", maskB, c_mask, s_c, (), ["maskB"])
    DMA("sp", iotaF, c_iota, s_c, (), ["iotaF"])
    DMA("sp", tokid, c_tokid, s_c, (), ["tokid"])
    epsT = A.take([2], F32)
    MS("dve", epsT[:, 0:1], LN_EPS, ["epsT"])
    MS("dve", epsT[:, 1:2], RMS_EPS, ["epsT"])
    MS("dve", onesF, 1.0, ["onesF"])
    MS("dve", onesB, 1.0, ["onesB"])
    negB = maskB
    TS("dve", negB, maskB, 1.0, 30000.0, ALU.subtract, ALU.mult, ["maskB"], ["maskB", "negB"])
    KEEP = A.off
    zrow = A.take([D], BF16)
    MS("dve", zrow, 0.0, ["zrow"])
    DMA("sp", x1b[S:S + 128, :], zrow, s_c, ["zrow"], ())
    p.barrier()
    route_holder = []

    def build_xT_block(src, b, xTb, xin_bufs, tagp):
        for ti in range(4):
            t = 4 * b + ti
            xin = xin_bufs[t % 2]
            xn = f"{tagp}xin{t % 2}"
            DMA("sp", xin, src[t * 128:(t + 1) * 128, :], SEM(xn), (), [xn])
            for half in range(2):
                bk = nbank()
                for q in range(4):
                    kc = half * 4 + q
                    TR(pb[bk][:, q * 128:(q + 1) * 128], xin[:, kc * 128:(kc + 1) * 128], identF,
                       [xn, "identF"], [pbn[bk]])
                CP("act" if half == 0 else "dve",
                   xTb[:, half * 4:half * 4 + 4, ti * 128:(ti + 1) * 128],
                   pb[bk].rearrange("p (q c) -> p q c", q=4), [pbn[bk]], [f"{tagp}xTb"])

    def phase_A0(src):
        A.reset(KEEP2)
        w_in = A.take([8, 1728], BF16)
        wkrs = A.take([8, 64], BF16)
        wq = A.take([3, 768], BF16)
        wqs = A.take([3, 4, 64], BF16)
        wkv = A.take([2, 1024], BF16)
        gq = A.take([3], F32)
        gkv = A.take([2], F32)
        cvec = A.take([3, 4], F32)
        cwrow = A.take([512], F32, parts=31)
        cw = A.take([4, 31], F32)
        cos = A.take([S], F32, parts=64)
        sin = A.take([S], F32, parts=64)
        s_w = SEM("wA")
        win_v = W["l0_w_in"].rearrange("(kc p) n -> p kc n", p=128)
        for kc in range(8):
            DMA("pool", w_in[:, kc, 0:864], win_v[:, kc, 0:864], s_w, (), ["w_in"])
            DMA("pool", w_in[:, kc, 864:1728], win_v[:, kc, 864:1728], s_w, (), ["w_in"])
            DMA("pool", wkrs[:, kc, 0:32], win_v[:, kc, 672:704], s_w, (), ["wkrs"])
            DMA("pool", wkrs[:, kc, 32:64], win_v[:, kc, 640:672], s_w, (), ["wkrs"])
        wq_v = W["l0_w_q_up"].rearrange("(kc p) n -> p kc n", p=128)
        for kc in range(3):
            DMA("pool", wq[:, kc, :], wq_v[:, kc, :], s_w, (), ["wq"])
            for h in range(4):
                c0 = h * 192 + 128
                DMA("pool", wqs[:, kc, h, 0:32], wq_v[:, kc, c0 + 32:c0 + 64], s_w, (), ["wqs"])
                DMA("pool", wqs[:, kc, h, 32:64], wq_v[:, kc, c0:c0 + 32], s_w, (), ["wqs"])
        wkv_v = W["l0_w_kv_up"].rearrange("(kc p) n -> p kc n", p=128)
        for kc in range(2):
            DMA("pool", wkv[:, kc, :], wkv_v[:, kc, :], s_w, (), ["wkv"])
        colvec(gq, W["l0_q_norm_g"], 3, s_w, "gq")
        colvec(gkv, W["l0_kv_norm_g"], 2, s_w, "gkv")
        colvec(cvec[:, 0, :], W["l0_conv_b"], 4, s_w, "cvec")
        colvec(cvec[:, 1, :], W["l0_conv_ln_g"], 4, s_w, "cvec")
        colvec(cvec[:, 2, :], W["l0_conv_ln_b"], 4, s_w, "cvec")
        DMA("sp", cwrow, W["l0_conv_w"], s_w, (), ["cwrow"])
        DMA("sp", cos, c_cos64, s_w, (), ["cos"])
        DMA("sp", sin, c_sin64, s_w, (), ["sin"])
        for cc in range(4):
            bk = nbank()
            TR(pb[bk][:, 0:31], cwrow[:, cc * 128:(cc + 1) * 128], identF[0:31, 0:31], ["cwrow", "identF"], [pbn[bk]])
            CP("dve", cw[:, cc, :], pb[bk][:, 0:31], [pbn[bk]], ["cw"])

        dg = A.take([4, 31, 128], BF16)
        for cc in range(4):
            for j in range(31):
                TS("dve", dg[:, cc, j, :], identF, cw[:, cc, j:j + 1], None, ALU.mult, None, ["identF", "cw"], ["dg"])
        xin_bufs = [A.take([D], F32), A.take([D], F32)]
        xTb = A.take([8, 512], BF16)
        ubuf = A.take([4, 542], BF16)
        ybuf = A.take([4, 512], F32)
        t512 = [A.take([512], F32) for _ in range(6)]
        qlT = A.take([3, 512], BF16)
        kvlT = A.take([2, 512], BF16)
        rstdq = A.take([512], F32)
        rstdkv = A.take([512], F32)
        rstdkv_tok = A.take([4], F32)
        mean_t = A.take([512], F32)
        rstd_t = A.take([512], F32)
        ob = [A.take([512], BF16) for _ in range(4)]
        vxt = [A.take([4, 129], BF16) for _ in range(2)]
        MS("dve", ubuf, 0.0, ["ubuf0", "ubuf1", "ubuf2", "ubuf3"])
        for i in range(2):
            MS("dve", vxt[i], 1.0, [f"vxt{i}"])
        obi = [0]

        def out_store(dst, src_tile_fn, parts=128):
            k = obi[0] % 4
            obi[0] += 1
            tn = f"ob{k}"
            src_tile_fn(ob[k][0:parts, :], tn)
            DMA("sp", dst, ob[k][0:parts, :], SEM(tn), [tn], ())

        for b in range(NB):
            t0 = b * 512
            build_xT_block(src, b, xTb, xin_bufs, "A")
            xr = ["AxTb"]

            def proj(col0, m, wt=w_in, wn="w_in"):
                bk = nbank()
                for kc in range(8):
                    MM(pb[bk][0:m, :], wt[:, kc, col0:col0 + m], xTb[:, kc, :], kc == 0, kc == 7,
                       xr + [wn], [pbn[bk]])
                return bk

            for cc in range(4):
                un = f"ubuf{cc}"
                ba = proj(704 + cc * 128, 128)
                bg = proj(1216 + cc * 128, 128)
                sg = t512[0]
                ACT(sg, pb[bg], AF.Sigmoid, [pbn[bg]], ["t0"])
                TT("dve", ubuf[:, cc, 30:542], pb[ba], sg, ALU.mult, [pbn[ba], "t0"], [un])
                yn = f"y{cc}"
                bc = nbank()
                for j in range(31):
                    MM(pb[bc], dg[:, cc, j, :], ubuf[:, cc, j:j + 512], j == 0, j == 30, ["dg", un], [pbn[bc]])
                ACT(ybuf[:, cc, :], pb[bc], AF.Identity, [pbn[bc], "cvec"], [yn], bias=cvec[:, 0, cc:cc + 1])
                CP("pool", ubuf[:, cc, 0:30], ubuf[:, cc, 512:542], [un], [un])
            b1 = nbank()
            b2 = nbank()
            for cc in range(4):
                MM(pb[b1], onesF, ybuf[:, cc, :], cc == 0, cc == 3, ["onesF", f"y{cc}"], [pbn[b1]])
            for cc in range(4):
                sq = t512[1 + cc % 2]
                sqn = f"t{1 + cc % 2}"
                TT("pool", sq, ybuf[:, cc, :], ybuf[:, cc, :], ALU.mult, [f"y{cc}"], [sqn])
                MM(pb[b2], onesF, sq, cc == 0, cc == 3, ["onesF", sqn], [pbn[b2]])
            ACT(mean_t, pb[b1], AF.Identity, [pbn[b1]], ["mean_t"], scale=1.0 / 512)
            TT("pool", t512[3], mean_t, mean_t, ALU.mult, ["mean_t"], ["t3"])
            STT("dve", rstd_t, pb[b2], 1.0 / 512, t512[3], ALU.mult, ALU.subtract, [pbn[b2], "t3"], ["rstd_t"])
            RSQRT(rstd_t, rstd_t, 1.0, LN_EPS, ["rstd_t"], ["rstd_t"])
            for cc in range(4):
                TT("pool", t512[4], ybuf[:, cc, :], mean_t, ALU.subtract, [f"y{cc}", "mean_t"], ["t4"])
                TT("pool", t512[5], t512[4], rstd_t, ALU.mult, ["t4", "rstd_t"], ["t5"])

                def fill(o, tn, cc=cc):
                    ACT(o, t512[5], AF.Silu, ["t5", "cvec"], [tn], bias=cvec[:, 2, cc:cc + 1],
                        scale=cvec[:, 1, cc:cc + 1])
                out_store(convT[cc][:, t0:t0 + 512], fill)

            def lat(col0, nch, lT, lTn, g, gn, rstd, rstdn, dim):
                bss = nbank()
                for mc in range(nch):
                    bk = proj(col0 + mc * 128, 128)
                    ACT(lT[:, mc, :], pb[bk], AF.Identity, [pbn[bk], gn], [lTn], scale=g[:, mc:mc + 1])
                    sq = t512[mc % 2]
                    sqn = f"t{mc % 2}"
                    TT("dve", sq, pb[bk], pb[bk], ALU.mult, [pbn[bk]], [sqn]) if False else \
                        ACT(sq, pb[bk], AF.Square, [pbn[bk]], [sqn])
                    MM(pb[bss], onesF, sq, mc == 0, mc == nch - 1, ["onesF", sqn], [pbn[bss]])
                    if lTn == "kvlT":
                        for ti in range(4):
                            MM(pb[7][:, ti * 2:ti * 2 + 1], sq[:, ti * 128:(ti + 1) * 128], onesF[:, 0:1],
                               mc == 0 and ti == 0, mc == nch - 1 and ti == 3, [sqn, "onesF"], ["pb7"], skip=True)
                RSQRT(rstd, pb[bss], 1.0 / dim, RMS_EPS, [pbn[bss]], [rstdn])

            lat(0, 3, qlT, "qlT", gq, "gq", rstdq, "rstdq", 384)
            lat(384, 2, kvlT, "kvlT", gkv, "gkv", rstdkv, "rstdkv", 256)
            RSQRT(rstdkv_tok, pb[7].rearrange("p (a b) -> p a b", b=2)[:, 0:4, 0], 1.0 / 256, RMS_EPS, ["pb7"], ["rkt"])

            def rope_store(bm, bs, dst, rstd=None, rstdn=None):
                TT("dve", t512[0][0:64], pb[bm][0:64], cos[:, t0:t0 + 512], ALU.mult, [pbn[bm], "cos"], ["t0"])
                TT("dve", t512[1][0:64], pb[bs][0:64], sin[:, t0:t0 + 512], ALU.mult, [pbn[bs], "sin"], ["t1"])

                def fill(o, tn):
                    if rstd is None:
                        TT("pool", o, t512[0][0:64], t512[1][0:64], ALU.add, ["t0", "t1"], [tn])
                    else:
                        TT("pool", t512[2][0:64], t512[0][0:64], t512[1][0:64], ALU.add, ["t0", "t1"], ["t2"])
                        TT("pool", o, t512[2][0:64], rstd[0:64], ALU.mult, ["t2", rstdn], [tn])
                out_store(dst, fill, parts=64)

            bm = proj(640, 64)
            bs = proj(0, 64, wkrs, "wkrs")
            rope_store(bm, bs, KrT[:, t0:t0 + 512])

            for h in range(4):
                bk = nbank()
                for mc in range(3):
                    MM(pb[bk], wq[:, mc, h * 192:h * 192 + 128], qlT[:, mc, :], mc == 0, mc == 2,
                       ["wq", "qlT"], [pbn[bk]])

                def fill(o, tn, bk=bk):
                    TT("dve", o, pb[bk], rstdq, ALU.mult, [pbn[bk], "rstdq"], [tn])
                out_store(QnT[h][:, t0:t0 + 512], fill)
                bm = nbank()
                bs = nbank()
                for mc in range(3):
                    MM(pb[bm][0:64], wq[:, mc, h * 192 + 128:h * 192 + 192], qlT[:, mc, :], mc == 0, mc == 2,
                       ["wq", "qlT"], [pbn[bm]])
                for mc in range(3):
                    MM(pb[bs][0:64], wqs[:, mc, h, :], qlT[:, mc, :], mc == 0, mc == 2, ["wqs", "qlT"], [pbn[bs]])
                rope_store(bm, bs, QrT[h][:, t0:t0 + 512], rstdq, "rstdq")

            for h in range(4):
                bk = nbank()
                for mc in range(2):
                    MM(pb[bk], wkv[:, mc, h * 256:h * 256 + 128], kvlT[:, mc, :], mc == 0, mc == 1,
                       ["wkv", "kvlT"], [pbn[bk]])

                def fill(o, tn, bk=bk):
                    TT("dve", o, pb[bk], rstdkv, ALU.mult, [pbn[bk], "rstdkv"], [tn])
                out_store(KnT[h][:, t0:t0 + 512], fill)
            for ti in range(4):
                t = 4 * b + ti
                bk = nbank()
                for h in range(4):
                    for mc in range(2):
                        MM(pb[bk][:, h * 128:(h + 1) * 128], kvlT[:, mc, ti * 128:(ti + 1) * 128],
                           wkv[:, mc, h * 256 + 128:h * 256 + 256], mc == 0 and h == 0, mc == 1 and h == 3,
                           ["kvlT", "wkv"], [pbn[bk]], skip=True)
                vt = vxt[t % 2]
                vn = f"vxt{t % 2}"
                TS("dve", vt[:, :, 0:128], pb[bk].rearrange("p (h c) -> p h c", h=4), rstdkv_tok[:, ti:ti + 1],
                   None, ALU.mult, None, [pbn[bk], "rkt"], [vn])
                DMA("sp", Vx0[:, t].rearrange("h p c -> p h c"), vt, SEM(vn), [vn], ())
        p.barrier()

    def attn_phase(nheads, maps_of_head, dv, scale, load_head, post, attnT, consts_extra=None):
        pT = [A.take([512], BF16) for _ in range(3)]
        for h in range(nheads):
            maps, vx, vn = load_head(h)
            steps = []
            for j in range(NB):
                for mi, parts in enumerate(maps):
                    for i in range(4 * j + 4):
                        steps.append((j, mi, parts, i))

            def emit_scores(n):
                j, mi, parts, i = steps[n]
                sb = 4 + n % 3
                diag = i - 4 * j >= 0
                for ci, (qa, ka, qn, kn) in enumerate(parts):
                    MM(pb[sb], ka[:, i * 128:(i + 1) * 128], qa[:, j * 512:(j + 1) * 512],
                       ci == 0, ci == len(parts) - 1 and not diag, [qn, kn], [pbn[sb]])
                if diag:
                    MM(pb[sb], identB, negB[:, i - 4 * j, :], False, True, ["identB", "negB"], [pbn[sb]])

            pending = []
            pend_at = [0]
            emit_scores(0)
            emit_scores(1)
            for n, (j, mi, parts, i) in enumerate(steps):
                if n + 2 < len(steps):
                    emit_scores(n + 2)
                sb = 4 + n % 3
                pt = pT[n % 3]
                ptn = f"pT{n % 3}"
                ACT(pt, pb[sb], AF.Exp, [pbn[sb]], [ptn], scale=scale)
                r = i - 4 * j
                for qs in range(4):
                    if r >= 0 and qs < r:
                        continue
                    MM(pb[qs][:, 0:dv + 1], pt[:, qs * 128:(qs + 1) * 128], vx[:, i, :],
                       i == 0, i == 4 * j + qs, [ptn, vn], [pbn[qs]])
                if pending and n - pend_at[0] >= 6:
                    for fn in pending:
                        fn()
                    pending.clear()
                if i == 4 * j + 3:
                    for fn in pending:
                        fn()
                    pending.clear()
                    pending.extend(post(h, mi, j) or [])
                    pend_at[0] = n
            for fn in pending:
                fn()
            pending.clear()

    def phase_B0(attnT):
        qn_b = [A.take([S], BF16) for _ in range(2)]
        kn_b = [A.take([S], BF16) for _ in range(2)]
        qr_b = [A.take([S], BF16, parts=64) for _ in range(2)]
        kr = A.take([S], BF16, parts=64)
        vx_b = [A.take([NT, 129], BF16) for _ in range(2)]
        rinv = A.take([4], F32)
        on = [A.take([128], BF16) for _ in range(4)]
        DMA("sp", kr, KrT, SEM("kr"), (), ["kr"])
        oi = [0]

        def load_head(h):
            s = h % 2
            sm = SEM(f"hd{s}")
            DMA("sp", qn_b[s], QnT[h], sm, (), [f"qn{s}"])
            DMA("sp", kn_b[s], KnT[h], sm, (), [f"kn{s}"])
            DMA("sp", qr_b[s], QrT[h], sm, (), [f"qr{s}"])
            DMA("sp", vx_b[s], Vx0[h].rearrange("t p c -> p t c"), sm, (), [f"vx{s}"])
            maps = [[(qn_b[s], kn_b[s], f"qn{s}", f"kn{s}"), (qr_b[s], kr, f"qr{s}", "kr")]]
            return maps, vx_b[s], f"vx{s}"

        def post(h, mi, j):
            for qs in range(4):
                p.c("dve", lambda e, qs=qs: e.reciprocal(rinv[:, qs:qs + 1], pb[qs][:, 128:129]), [pbn[qs]], ["rinv"])
                TS("dve", on[qs], pb[qs][:, 0:128], rinv[:, qs:qs + 1], None, ALU.mult, None, [pbn[qs], "rinv"], [f"on{qs}"])

            def later():
                pv = pb[7].bitcast(BF16)
                for qs in range(4):
                    TR(pv[:, qs * 128:(qs + 1) * 128], on[qs], identB, [f"on{qs}", "identB"], ["pb7"])
                CP("dve", attnT[:, h, j * 512:(j + 1) * 512], pv[:, 0:512], ["pb7"], ["attnT"])
            return [later]

        attn_phase(4, None, 128, 192.0 ** -0.5, load_head, post, attnT)

    def phase_A1(src):
        A.reset(KEEP2)
        wqkv = A.take([8, 3072], BF16)
        wsw = A.take([8, 16, 128], BF16)
        cos = A.take([S], F32)
        sin = A.take([S], F32)
        s_w = SEM("wA")
        wv = W["l1_w_qkv"].rearrange("(kc p) n -> p kc n", p=128)
        for kc in range(8):
            for c3 in range(3):
                DMA("pool", wqkv[:, kc, c3 * 1024:(c3 + 1) * 1024], wv[:, kc, c3 * 1024:(c3 + 1) * 1024], s_w, (), ["wqkv"])
            sv = wv[:, kc, 0:2048].rearrange("p (hb two c) -> p hb two c", two=2, c=64)
            DMA("pool", wsw[:, kc, :, 0:64], sv[:, :, 1, :], s_w, (), ["wsw"])
            DMA("pool", wsw[:, kc, :, 64:128], sv[:, :, 0, :], s_w, (), ["wsw"])
        DMA("sp", cos, c_cos128, s_w, (), ["cos"])
        DMA("sp", sin, c_sin128, s_w, (), ["sin"])
        xin_bufs = [A.take([D], F32), A.take([D], F32)]
        xTb = A.take([8, 512], BF16)
        t1 = [A.take([512], F32) for _ in range(2)]
        t2 = [A.take([512], F32) for _ in range(2)]
        ob = [A.take([512], BF16) for _ in range(4)]
        vxt = [A.take([4, 257], BF16) for _ in range(2)]
        for i in range(2):
            MS("dve", vxt[i], 1.0, [f"vxt{i}"])
        cnt = [0]
        for b in range(NB):
            t0 = b * 512
            build_xT_block(src, b, xTb, xin_bufs, "A")
            for hb in range(16):
                bm = nbank()
                bs = nbank()
                for kc in range(8):
                    MM(pb[bm], wqkv[:, kc, hb * 128:(hb + 1) * 128], xTb[:, kc, :], kc == 0, kc == 7, ["wqkv", "AxTb"], [pbn[bm]])
                for kc in range(8):
                    MM(pb[bs], wsw[:, kc, hb, :], xTb[:, kc, :], kc == 0, kc == 7, ["wsw", "AxTb"], [pbn[bs]])
                k = cnt[0] % 2
                o = cnt[0] % 4
                cnt[0] += 1
                TT("dve", t1[k], pb[bm], cos[:, t0:t0 + 512], ALU.mult, [pbn[bm], "cos"], [f"t1{k}"])
                TT("dve", t2[k], pb[bs], sin[:, t0:t0 + 512], ALU.mult, [pbn[bs], "sin"], [f"t2{k}"])
                TT("pool", ob[o], t1[k], t2[k], ALU.add, [f"t1{k}", f"t2{k}"], [f"ob{o}"])
                dst = QT1[hb] if hb < 8 else KT1[hb - 8]
                DMA("sp", dst[:, t0:t0 + 512], ob[o], SEM(f"ob{o}"), [f"ob{o}"], ())
            for ti in range(4):
                t = 4 * b + ti
                vt = vxt[t % 2]
                vn = f"vxt{t % 2}"
                for half in range(2):
                    bk = nbank()
                    for kc in range(8):
                        MM(pb[bk], xTb[:, kc, ti * 128:(ti + 1) * 128], wqkv[:, kc, 2048 + half * 512:2048 + (half + 1) * 512],
                           kc == 0, kc == 7, ["AxTb", "wqkv"], [pbn[bk]])
                    CP("act", vt[:, 2 * half:2 * half + 2, 0:256], pb[bk].rearrange("p (h c) -> p h c", h=2), [pbn[bk]], [vn])
                DMA("sp", Vx1[:, t].rearrange("h p c -> p h c"), vt, SEM(vn), [vn], ())
        p.barrier()

    def phase_B1(attnT):
        li = 0.8 - 0.6 * math.exp(-0.3)
        q_b = [[A.take([S], BF16) for _ in range(2)] for _ in range(2)]
        k_b = [[A.take([S], BF16) for _ in range(2)] for _ in range(2)]
        vx_b = [A.take([NT, 257], BF16) for _ in range(2)]
        lam4 = A.take([4, 128], F32)
        lp = A.take([2, 128], F32)
        ls = A.take([2], F32)
        neglam = A.take([1], F32)
        gsub = A.take([256], F32)
        o1 = A.take([4, 256], F32)
        o2 = A.take([4, 256], F32)
        dd = A.take([256], F32)
        sqj = A.take([256], F32)
        ss = A.take([1], F32)
        rinv = A.take([4], F32)
        rs = A.take([1], F32)
        on = [A.take([256], BF16) for _ in range(4)]
        s_w = SEM("wB")
        for i, n in enumerate(("lambda_q1", "lambda_k1", "lambda_q2", "lambda_k2")):
            DMA("sp", lam4[:, i, :], W["l1_" + n].partition_broadcast(128), s_w, (), ["lam4"])
        DMA("sp", gsub, W["l1_subln_g"].partition_broadcast(128), s_w, (), ["gsub"])
        for m in range(2):
            TT("dve", lp[:, m, :], lam4[:, 2 * m, :], lam4[:, 2 * m + 1, :], ALU.mult, ["lam4"], ["lp"])
        p.c("dve", lambda e: e.tensor_reduce(ls, lp, AX.X, ALU.add), ["lp"], ["ls"])
        ACT(ls, ls, AF.Exp, ["ls"], ["ls"])
        TT("dve", neglam, ls[:, 1:2], ls[:, 0:1], ALU.subtract, ["ls"], ["neglam"])
        TS("dve", neglam, neglam, -li, None, ALU.add, None, ["neglam"], ["neglam"])
        TS("dve", gsub, gsub, 1.0 - li, None, ALU.mult, None, ["gsub"], ["gsub"])
        oi = [0]

        def load_head(h):
            s = h % 2
            sm = SEM(f"hd{s}")
            maps = []
            for m in range(2):
                DMA("sp", q_b[s][m], QT1[2 * h + m], sm, (), [f"q{s}{m}"])
                DMA("sp", k_b[s][m], KT1[2 * h + m], sm, (), [f"k{s}{m}"])
                maps.append([(q_b[s][m], k_b[s][m], f"q{s}{m}", f"k{s}{m}")])
            DMA("sp", vx_b[s], Vx1[h].rearrange("t p c -> p t c"), sm, (), [f"vx{s}"])
            return maps, vx_b[s], f"vx{s}"

        def post(h, mi, j):
            dst = o1 if mi == 0 else o2
            dn_ = "o1" if mi == 0 else "o2"
            for qs in range(4):
                p.c("dve", lambda e, qs=qs: e.reciprocal(rinv[:, qs:qs + 1], pb[qs][:, 256:257]), [pbn[qs]], ["rinv"])
                TS("dve", dst[:, qs, :], pb[qs][:, 0:256], rinv[:, qs:qs + 1], None, ALU.mult, None, [pbn[qs], "rinv"], [f"{dn_}{qs}"])
            if mi == 0:
                return []
            for qs in range(4):
                STT("dve", dd, o2[:, qs, :], neglam, o1[:, qs, :], ALU.mult, ALU.add, [f"o2{qs}", "neglam", f"o1{qs}"], ["dd"])
                ACT(sqj, dd, AF.Square, ["dd"], ["sqj", "ss"], accum=ss)
                RSQRT(rs, ss, 1.0 / 256, RMS_EPS, ["ss"], ["rs"])
                STT("dve", on[qs], dd, rs, gsub, ALU.mult, ALU.mult, ["dd", "rs", "gsub"], [f"on{qs}"])

            def later():
                for qs in range(4):
                    t = 4 * j + qs
                    pv = pb[7].bitcast(BF16)
                    for c in range(2):
                        TR(pv[:, c * 128:(c + 1) * 128], on[qs][:, c * 128:(c + 1) * 128], identB, [f"on{qs}", "identB"], ["pb7"])
                    CP("dve", attnT[:, 2 * h:2 * h + 2, t * 128:(t + 1) * 128],
                       pv[:, 0:256].rearrange("p (c q) -> p c q", c=2), ["pb7"], ["attnT"])
            return [later]

        attn_phase(4, None, 256, 128.0 ** -0.5, load_head, post, attnT)

    def phase_C(l, attnT, nattn, conv_src, xres, route):
        pre = f"l{l}_"
        wo = A.take([8, D], BF16)
        s_w = SEM("wC")
        wo_v = W[pre + "w_o"].rearrange("(kc p) n -> p kc n", p=128)
        for kc in range(8):
            DMA("pool", wo[:, kc, :], wo_v[:, kc, :], s_w, (), ["wo"])
        cat = [(attnT[:, c, :], "attnT") for c in range(nattn)]
        if conv_src is not None:
            cvT = A.take([4, S], BF16)
            for cc in range(4):
                DMA("sp", cvT[:, cc, :], conv_src[cc], s_w, (), ["cvT"])
            cat += [(cvT[:, cc, :], "cvT") for cc in range(4)]
        gB = A.take([D], F32)
        bB = A.take([D], F32)
        DMA("sp", gB, W[pre + "ln1_g"].partition_broadcast(128), s_w, (), ["gB"])
        DMA("sp", bB, W[pre + "ln1_b"].partition_broadcast(128), s_w, (), ["bB"])
        rw = A.take([8, E], F32)
        DMA("sp", rw, W[pre + "router_w"].rearrange("(kc p) n -> p kc n", p=128), s_w, (), ["rw"])
        rbB = A.take([E], F32)
        DMA("sp", rbB, W[pre + "router_b"].partition_broadcast(128), s_w, (), ["rbB"])
        xin = [A.take([D], F32) for _ in range(2)]
        hb = [A.take([D], F32) for _ in range(2)]
        x1t = [A.take([D], F32) for _ in range(2)]
        x1bt = [A.take([D], BF16) for _ in range(2)]
        x1T = A.take([8, 128], F32)
        stats = A.take([2, 6], F32)
        mv = A.take([2], F32)
        rstd = A.take([1], F32)
        prev = None
        for t in range(NT):
            s = t % 2
            DMA("sp", xin[s], xres[t * 128:(t + 1) * 128, :], SEM(f"Cxin{s}"), (), [f"xin{s}"])
            bks = [nbank(0, 5), nbank(0, 5)]
            for half in range(2):
                for c, (ca, cn) in enumerate(cat):
                    MM(pb[bks[half]], ca[:, t * 128:(t + 1) * 128], wo[:, c, half * 512:(half + 1) * 512],
                       c == 0, c == len(cat) - 1, [cn, "wo"], [pbn[bks[half]]])
            hn = f"hb{s}"
            for half in range(2):
                STT("dve", hb[s][:, half * 512:(half + 1) * 512], xin[s][:, half * 512:(half + 1) * 512], DN_ALPHA,
                    pb[bks[half]], ALU.mult, ALU.add, [f"xin{s}", pbn[bks[half]]], [hn])
            layer_norm_tile(hb[s], hn, x1t[s], f"x1t{s}", gB, bB, stats, mv, rstd)
            DMA("sp", x1f[t * 128:(t + 1) * 128, :], x1t[s], SEM(f"Cx1f{s}"), [f"x1t{s}"], ())
            CP("act", x1bt[s], x1t[s], [f"x1t{s}"], [f"x1bt{s}"])
            DMA("sp", x1b[t * 128:(t + 1) * 128, :], x1bt[s], SEM(f"Cx1b{s}"), [f"x1bt{s}"], ())
            for half in range(2):
                bk = nbank(0, 5)
                for q in range(4):
                    kc = half * 4 + q
                    TR(pb[bk][:, q * 128:(q + 1) * 128], x1t[s][:, kc * 128:(kc + 1) * 128], identF,
                       [f"x1t{s}", "identF"], [pbn[bk]])
                CP("act", x1T[:, half * 4:half * 4 + 4, :], pb[bk].rearrange("p (q c) -> p q c", q=4),
                   [pbn[bk]], ["x1T"])
            bk = 5 + t % 2
            for kc in range(8):
                MM(pb[bk][:, 0:E], x1T[:, kc, :], rw[:, kc, :], kc == 0, kc == 7, ["x1T", "rw"], [pbn[bk]])
            if prev is not None:
                route.tile(*prev)
            prev = (t, pb[bk][:, 0:E], pbn[bk], rbB)
        route.tile(*prev)
        p.barrier()

    def layer_norm_tile(h, hn, out, on, gB, bB, stats, mv, rstd):
        for c in range(2):
            p.c("dve", lambda e, c=c: e.bn_stats(stats[:, c, :], h[:, c * 512:(c + 1) * 512]), [hn], ["stats"])
        p.c("dve", lambda e: e.bn_aggr(mv, stats.rearrange("p a b -> p (a b)")), ["stats"], ["mv"])
        RSQRT(rstd, mv[:, 1:2], 1.0, LN_EPS, ["mv"], ["rstd"])
        TS("dve", out, h, mv[:, 0:1], rstd, ALU.subtract, ALU.mult, [hn, "mv", "rstd"], [on])
        TT("dve", out, out, gB, ALU.mult, [on, "gB"], [on])
        TT("dve", out, out, bB, ALU.add, [on, "bB"], [on])

    class Route:
        def __init__(self):
            self.gates = A.take([NT, 4], F32)
            self.eidx = A.take([NT, 4], F32)
            self.yrow = A.take([NT, 4], I32)
            self.cum = A.take([E], BF16)
            self.logit = A.take([E], F32)
            self.mx = A.take([8], F32)
            self.mi = A.take([8], U32)
            self.mask = A.take([E], BF16)
            self.ex = A.take([4], F32)
            self.sm = A.take([1], F32)
            self.rank = A.take([E], F32)
            self.oh = A.take([E], F32)
            self.rk = A.take([4], F32)
            self.ri = A.take([4], I32)
            self.t1 = A.take([4], I32)
            self.t2 = A.take([4], I32)
            self.fl = A.take([NT, 4], I32)
            self.ef = A.take([4], F32)
            self.negm = A.take([1], F32)
            self.mm = A.take([4], F32)
            self.qq = A.take([4], F32)
            self.yf = A.take([4], F32)
            self.ff = A.take([4], F32)
            self.zt = A.take([E * NJMAX], I32)
            self.end = A.off

        def init(self, cap):
            self.cap = cap
            self.nj = cap // 128
            MS("dve", self.cum, 0.0, ["cum"])
            DMA("sp", self.zt, c_zero, SEM("zt"), (), ["zt"])
            DMA("sp", tokidx.rearrange("(p c) o -> p (c o)", p=128), self.zt, SEM("zt"), ["zt"], ["tokidx"])


        def tile(self, t, lg_ps, lgn, rbB):
            r = self
            TT("dve", r.logit, lg_ps, rbB, ALU.add, [lgn, "rbB"], ["logit"])
            p.c("dve", lambda e: e.max(r.mx, r.logit), ["logit"], ["mx"])
            p.c("dve", lambda e: e.max_index(r.mi, r.mx, r.logit), ["logit", "mx"], ["mi"])
            TS("dve", r.mask, r.logit, r.mx[:, 3:4], None, ALU.is_ge, None, ["logit", "mx"], ["mask"])
            TS("dve", r.negm, r.mx[:, 0:1], -1.0, None, ALU.mult, None, ["mx"], ["negm"])
            ACT(r.ex, r.mx[:, 0:4], AF.Exp, ["mx", "negm"], ["ex"], bias=r.negm)
            p.c("dve", lambda e: e.tensor_reduce(r.sm, r.ex, AX.X, ALU.add), ["ex"], ["sm"])
            p.c("dve", lambda e: e.reciprocal(r.sm, r.sm), ["sm"], ["sm"])
            TS("dve", r.gates[:, t, :], r.ex, r.sm, None, ALU.mult, None, ["ex", "sm"], ["gates"])
            CP("dve", r.ef, r.mi[:, 0:4], ["mi"], ["ef"])
            CP("pool", r.eidx[:, t, :], r.ef, ["ef"], ["eidx"])
            bk = nbank(0, 5)
            MM(pb[bk][:, 0:E], ltriB, r.mask, True, False, ["ltriB", "mask"], [pbn[bk]])
            MM(pb[bk][:, 0:E], onesB, r.cum, False, True, ["onesB", "cum"], [pbn[bk]])
            CP("dve", r.rank, pb[bk][:, 0:E], [pbn[bk]], ["rank"])
            TT("pool", r.cum, r.cum, r.mask, ALU.add, ["cum", "mask"], ["cum"])
            for k in range(4):
                TS("dve", r.oh, iotaF[:, 0, :], r.ef[:, k:k + 1], None, ALU.is_equal, None, ["iotaF", "ef"], ["oh"])
                TT("dve", r.oh, r.oh, r.rank, ALU.mult, ["oh", "rank"], ["oh"])
                p.c("dve", lambda e, k=k: e.tensor_reduce(r.rk[:, k:k + 1], r.oh, AX.X, ALU.add), ["oh"], ["rk"])
            TS("dve", r.qq, r.rk, 128.0, None, ALU.is_ge, None, ["rk"], ["qq"])
            for i in range(2, r.nj):
                STT("dve", r.qq, r.rk, 128.0 * i, r.qq, ALU.is_ge, ALU.add, ["rk", "qq"], ["qq"])
            STT("dve", r.mm, r.qq, -128.0, r.rk, ALU.mult, ALU.add, ["rk", "qq"], ["mm"])
            STT("dve", r.ff, r.mm, float(E * r.nj), r.qq, ALU.mult, ALU.add, ["mm", "qq"], ["ff"])
            STT("dve", r.ff, r.ef, float(r.nj), r.ff, ALU.mult, ALU.add, ["ef", "ff"], ["ff"])
            TS("dve", r.ff, r.ff, float(128 * E * r.nj - 1), None, ALU.min, None, ["ff"], ["ff"])
            CP("dve", r.fl[:, t, :], r.ff, ["ff"], [f"fl{t}"])
            for k in range(4):
                SCATTER(tokidx, tokid[:, t, k:k + 1], r.fl[:, t, k:k + 1], SEM(f"sc{k}"), [f"fl{t}", "tokid", "tokidx"], [f"tokidx_{t}_{k}"])

    def phase_D(l, CAP):
        pre = f"l{l}_"
        NJ = CAP // 128
        NSL = 10
        NSTG = 6
        LAG = 3
        bgT = A.take([16, E], F32)
        bu1 = A.take([8, E], F32)
        mark = A.off
        bgrow = A.take([2 * D], F32, parts=E)
        DMA("sp", bgrow, W[pre + "b_gu"], SEM("bg"), (), ["bgrow"])
        for c in range(16):
            bk = nbank(0, 6)
            TR(pb[bk][:, 0:E], bgrow[:, c * 128:(c + 1) * 128], identF[0:E, 0:E], ["bgrow", "identF"], [pbn[bk]])
            CP("dve", bgT[:, c, :], pb[bk][:, 0:E], [pbn[bk]], ["bgT"])
        TS("dve", bu1, bgT[:, 8:16, :], 1.0, None, ALU.add, None, ["bgT"], ["bu1"])
        p.barrier()
        A.reset(mark)
        slots = [A.take([4, 1024], BF16) for _ in range(NSL)]
        stg = [A.take([1024], F32) for _ in range(NSTG)]
        idx = A.take([E * NJ], I32)
        DMA("sp", idx, tokidx[0:128 * E * NJ, :].rearrange("(p c) o -> p (c o)", p=128), SEM("idx"), (), ["idx"])
        tokx = A.take([E * NJ], I32)
        TS("dve", tokx, idx, 2, None, ALU.arith_shift_right, None, ["idx"], ["tokx"])
        xg = A.take([NJ, D], BF16)
        xgT = A.take([8, CAP], BF16)
        actT = A.take([8, CAP], BF16)
        gt = [A.take([CAP], F32) for _ in range(2)]
        st = [A.take([CAP], F32) for _ in range(2)]
        ut = [A.take([CAP], F32) for _ in range(2)]
        yst = [A.take([D], F32) for _ in range(2)]
        wgu_v = W[pre + "w_gu"]
        wdn_v = W[pre + "w_dn"]
        ucnt = [0]
        scnt = [0]
        fifo = []
        fcnt = [0]
        ycnt = [0]
        tcnt = [0]

        def open_units(n):
            r = []
            for _ in range(n):
                k = ucnt[0] % NSL
                ucnt[0] += 1
                r.append((slots[k], f"ws{k}"))
            return r

        def gu_chunks(e, units):
            ch = []
            for part in range(2):
                for kh in range(2):
                    wt, wn = units[part * 2 + kh]
                    for kc in range(4):
                        r0 = (kh * 4 + kc) * 128
                        ch.append((wt[:, kc, :], wn, wgu_v[e, r0:r0 + 128, part * 1024:(part + 1) * 1024]))
            return ch

        def dn_chunks(e, units):
            ch = []
            for kh in range(2):
                wt, wn = units[kh]
                for kc in range(4):
                    r0 = (kh * 4 + kc) * 128
                    ch.append((wt[:, kc, :], wn, wdn_v[e, r0:r0 + 128, :]))
            return ch

        def emit_cast():
            dst, wn, i = fifo.pop(0)
            CP("act", dst, stg[i], [f"stg{i}"], [wn])

        def emit_load(chunk):
            dst, wn, src = chunk
            i = scnt[0] % NSTG
            scnt[0] += 1
            DMA("sp", stg[i], src, SEM(f"stg{i}"), (), [f"stg{i}"])
            fifo.append((dst, wn, i))
            if len(fifo) > LAG:
                emit_cast()

        def gather_expert(e):
            for j in range(NJ):
                GATHER(xg[:, j, :], x1b, tokx[:, e * NJ + j:e * NJ + j + 1], SEM("xg"), ["tokx"], ["xg"])

        gu_units = open_units(4)
        dn_units = open_units(2)
        for ch in gu_chunks(0, gu_units) + dn_chunks(0, dn_units):
            emit_load(ch)
        while fifo:
            emit_cast()
        gather_expert(0)
        for e in range(E):
            nxt_gu_units = open_units(4) if e + 1 < E else None
            nxt_gu = gu_chunks(e + 1, nxt_gu_units) if e + 1 < E else []
            if e + 1 == E:
                while fifo:
                    emit_cast()
            for j in range(NJ):
                for half in range(2):
                    bk = 6 + tcnt[0] % 2
                    tcnt[0] += 1
                    pv = pb[bk].bitcast(BF16)
                    for q in range(4):
                        kc = half * 4 + q
                        TR(pv[:, q * 128:(q + 1) * 128], xg[:, j, kc * 128:(kc + 1) * 128], identB,
                           ["xg", "identB"], [pbn[bk]])
                    CP("act" if half == 0 else "dve", xgT[:, half * 4:half * 4 + 4, j * 128:(j + 1) * 128],
                       pv[:, 0:512].rearrange("p (q c) -> p q c", q=4), [pbn[bk]], ["xgT"])
            if e + 1 < E:
                gather_expert(e + 1)
            for fc in range(8):
                f = fcnt[0] % 2
                fcnt[0] += 1
                res = []
                for part in range(2):
                    b0 = nbank(0, 6)
                    b1 = nbank(0, 6)
                    for kc in range(8):
                        wt, wn = gu_units[part * 2 + kc // 4]
                        lhs = wt[:, kc % 4, fc * 128:(fc + 1) * 128]
                        MM(pb[b0], lhs, xgT[:, kc, 0:512], kc == 0, kc == 7, [wn, "xgT"], [pbn[b0]])
                        MM(pb[b1][:, 0:CAP - 512], lhs, xgT[:, kc, 512:CAP], kc == 0, kc == 7, [wn, "xgT"], [pbn[b1]])
                    res.append((b0, b1))
                (g0, g1), (u0, u1) = res
                gn, sn_, un = f"gt{f}", f"st{f}", f"ut{f}"
                for (bk, lo, hi) in ((g0, 0, 512), (g1, 512, CAP)):
                    TS("dve", gt[f][:, lo:hi], pb[bk][:, 0:hi - lo], bgT[:, fc, e:e + 1], LIMIT, ALU.add, ALU.min,
                       [pbn[bk], "bgT"], [gn])
                ACT(st[f], gt[f], AF.Sigmoid, [gn], [sn_], scale=SW_ALPHA)
                for (bk, lo, hi) in ((u0, 0, 512), (u1, 512, CAP)):
                    ACT(ut[f][:, lo:hi], pb[bk][:, 0:hi - lo], AF.Identity, [pbn[bk], "bu1"], [un], bias=bu1[:, fc, e:e + 1])
                TS("dve", ut[f], ut[f], LIMIT + 1.0, 1.0 - LIMIT, ALU.min, ALU.max, [un], [un])
                TT("dve", gt[f], gt[f], st[f], ALU.mult, [gn, sn_], [gn])
                TT("dve", actT[:, fc, :], gt[f], ut[f], ALU.mult, [gn, un], ["actT"])
                for ch in nxt_gu[2 * fc:2 * fc + 2]:
                    emit_load(ch)
            nxt_dn_units = open_units(2) if e + 1 < E else None
            nxt_dn = dn_chunks(e + 1, nxt_dn_units) if e + 1 < E else []
            for j in range(NJ):
                y = ycnt[0] % 2
                ycnt[0] += 1
                ynm = f"yst{y}"
                for half in range(2):
                    bk = nbank(0, 6)
                    for fc in range(8):
                        wt, wn = dn_units[fc // 4]
                        MM(pb[bk], actT[:, fc, j * 128:(j + 1) * 128], wt[:, fc % 4, half * 512:(half + 1) * 512],
                           fc == 0, fc == 7, ["actT", wn], [pbn[bk]])
                    CP("act", yst[y][:, half * 512:(half + 1) * 512], pb[bk], [pbn[bk]], [ynm])
                SCATTER(Ybuf, yst[y], idx[:, e * NJ + j:e * NJ + j + 1], SEM(f"ysc{y}"), [ynm, "idx"], ())
                for ch in nxt_dn[j * 8 // NJ:(j + 1) * 8 // NJ]:
                    emit_load(ch)
            gu_units, dn_units = nxt_gu_units, nxt_dn_units
        assert not fifo
        p.barrier()

    def phase_E(l, route, dst):
        pre = f"l{l}_"
        s_w = SEM("wE")
        gB = A.take([D], F32)
        bB = A.take([D], F32)
        DMA("sp", gB, W[pre + "ln2_g"].partition_broadcast(128), s_w, (), ["gB"])
        DMA("sp", bB, W[pre + "ln2_b"].partition_broadcast(128), s_w, (), ["bB"])
        bdn = A.take([D], F32, parts=E)
        DMA("sp", bdn, W[pre + "b_dn"], s_w, (), ["bdn"])
        ykt = [A.take([4, D], F32) for _ in range(2)]
        yk = [[ykt[s_][:, k_, :] for k_ in range(4)] for s_ in range(2)]
        xin = [A.take([D], F32) for _ in range(2)]
        acc = [A.take([D], F32) for _ in range(2)]
        outt = [A.take([D], F32) for _ in range(2)]
        G = A.take([E], F32)
        oh = A.take([E], F32)
        GT = A.take([128], F32, parts=E)
        stats = A.take([2, 6], F32)
        mv = A.take([2], F32)
        rstd = A.take([1], F32)
        def stage1(t):
            s = t % 2
            DMA("sp", xin[s], x1f[t * 128:(t + 1) * 128, :], SEM(f"Exin{s}"), (), [f"xin{s}"])
            DMA("sp", ykt[s], Ybuf[t * 512:(t + 1) * 512, :].rearrange("(p k) d -> p k d", k=4), SEM(f"yk{s}"), (),
                [f"yk{s}_{k}" for k in range(4)])
            for k in range(4):
                TS("dve", oh, iotaF[:, 0, :], route.eidx[:, t, k:k + 1], route.gates[:, t, k:k + 1],
                   ALU.is_equal, ALU.mult, ["iotaF", "eidx", "gates"], ["oh"])
                if k == 0:
                    CP("dve", G, oh, ["oh"], ["G"])
                else:
                    TT("dve", G, G, oh, ALU.add, ["G", "oh"], ["G"])
            bk = nbank()
            TR(pb[bk][0:E, 0:128], G, identF, ["G", "identF"], [pbn[bk]])
            CP("act", GT, pb[bk][0:E, 0:128], [pbn[bk]], ["GT"])
            bks = [nbank(), nbank()]
            for half in range(2):
                MM(pb[bks[half]], GT, bdn[:, half * 512:(half + 1) * 512], True, True, ["GT", "bdn"], [pbn[bks[half]]])
            return bks

        def stage2(t, bks):
            s = t % 2
            an = f"acc{s}"
            for half in range(2):
                STT("dve", acc[s][:, half * 512:(half + 1) * 512], xin[s][:, half * 512:(half + 1) * 512], DN_ALPHA,
                    pb[bks[half]], ALU.mult, ALU.add, [f"xin{s}", pbn[bks[half]]], [an])
            for k in range(4):
                STT("dve", acc[s], yk[s][k], route.gates[:, t, k:k + 1], acc[s], ALU.mult, ALU.add,
                    [f"yk{s}_{k}", "gates", an], [an])
            layer_norm_tile(acc[s], an, outt[s], f"outt{s}", gB, bB, stats, mv, rstd)
            DMA("sp", dst[t * 128:(t + 1) * 128, :], outt[s], SEM(f"Eout{s}"), [f"outt{s}"], ())

        nb_ = stage1(0)
        for t in range(NT):
            cur = nb_
            if t + 1 < NT:
                nb_ = stage1(t + 1)
            stage2(t, cur)
        p.barrier()

    A.reset(KEEP)
    route = Route()
    KEEP2 = route.end
    def finish():
        p.emit()
        return nc, p, A

    phase_A0(x_in)
    if stop_after == "A0":
        return finish()
    A.reset(KEEP2)
    attnT = A.take([4, S], BF16)
    mark = A.off
    phase_B0(attnT)
    p.barrier()
    if "attnD" in dbg:
        for h in range(4):
            DMA("sp", attnD[h], attnT[:, h, :], SEM("dbg"), ["attnT"], ())
    if stop_after == "B0":
        return finish()
    A.reset(mark)
    route.init(CAPS[0])
    phase_C(0, attnT, 4, convT, x_in, route)
    if stop_after == "C0":
        return finish()
    A.reset(KEEP2)
    phase_D(0, CAPS[0])
    A.reset(KEEP2)
    phase_E(0, route, x2f if stop_after != "E0" else out_ap)
    if stop_after == "E0":
        return finish()
    phase_A1(x2f)
    if stop_after == "A1":
        return finish()
    A.reset(KEEP2)
    attnT1 = A.take([8, S], BF16)
    mark = A.off
    phase_B1(attnT1)
    p.barrier()
    if "attnD" in dbg:
        for c in range(8):
            DMA("sp", attnD[c], attnT1[:, c, :], SEM("dbg"), ["attnT"], ())
    if stop_after == "B1":
        return finish()
    A.reset(mark)
    route.init(CAPS[1])
    phase_C(1, attnT1, 8, None, x2f, route)
    A.reset(KEEP2)
    phase_D(1, CAPS[1])
    A.reset(KEEP2)
    phase_E(1, route, out_ap)
    return finish()


def make_consts():
    c = {}
    c["c_ident"] = np.eye(128, dtype=np.float32)
    tp = np.arange(128)
    c["c_ltri"] = (tp[:, None] < tp[None, :]).astype(np.float32)
    q = np.arange(512)
    m = np.zeros((128, 4, 512), np.float32)
    for r in range(4):
        m[:, r, :] = ((128 * r + tp[:, None]) <= q[None, :]).astype(np.float32)
    c["c_mask"] = m
    pos = np.arange(S, dtype=np.float32)

    def tables(dim):
        inv = (1.0 / (np.float32(10000.0) ** (np.arange(0, dim, 2, dtype=np.float32) / np.float32(dim)))).astype(np.float32)
        ang = (pos[None, :] * inv[:, None]).astype(np.float32)
        cs = np.cos(ang).astype(np.float32)
        sn = np.sin(ang).astype(np.float32)
        return np.concatenate([cs, cs], 0), np.concatenate([-sn, sn], 0)
    c["c_cos64"], c["c_sin64"] = tables(64)
    c["c_cos128"], c["c_sin128"] = tables(128)
    io = np.zeros((128, 3, 32), np.float32)
    io[:, 0, :] = np.arange(32)
    c["c_iota"] = io
    tk = (np.arange(NT)[None, :] * 128 + np.arange(128)[:, None]).astype(np.int32)
    c["c_tokid"] = (4 * tk[:, :, None] + np.arange(4)[None, None, :]).astype(np.int32)
    c["c_zero"] = np.full((128, E * NJMAX), 4 * S, np.int32)
    return {k: np.ascontiguousarray(v) for k, v in c.items()}


_CACHE = {}


def kernel(**inputs):
    if "nc" not in _CACHE:
        _CACHE["nc"] = build_program()[0]
    nc = _CACHE["nc"]
    consts = make_consts()
    x = np.asarray(inputs["x"], dtype=np.float32)
    shared = {k: np.ascontiguousarray(np.asarray(v)) for k, v in inputs.items() if k != "x"}
    in_maps = []
    for b in range(8):
        m = {"x": np.ascontiguousarray(x[b])}
        m.update(shared)
        m.update(consts)
        in_maps.append(m)
    res = run_bass_kernel_spmd(nc, in_maps, core_ids=list(range(8)))
    return np.stack([np.asarray(r["out"]) for r in res.results], 0).astype(np.float32)
```

```python
import contextlib
import math
import numpy as np
import concourse.bass as bass
import concourse.mybir as mybir
from concourse.bass_utils import run_bass_kernel_spmd

F32 = mybir.dt.float32
BF16 = mybir.dt.bfloat16
I32 = mybir.dt.int32
U32 = mybir.dt.uint32
U8 = mybir.dt.uint8
AF = mybir.ActivationFunctionType
ALU = mybir.AluOpType
AX = mybir.AxisListType
DSZ = {F32: 4, BF16: 2, I32: 4, U32: 4, U8: 1}

COMPUTE = ("pe", "dve", "act", "pool")
ISSUERS = ("pe", "dve", "act", "pool", "sp")

S = 4096
D = 1024
NT = 32
NB = 8
E = 32
CAPS = (768, 1024)
CAPMAX = 1024
NJMAX = CAPMAX // 128
NSLOT = E * CAPMAX
DN_ALPHA = 4.0 ** 0.25
LN_EPS = 1e-5
RMS_EPS = 1e-6
LIMIT = 7.0
SW_ALPHA = 1.702


class Sem:
    def __init__(self, handle, name, step):
        self.handle, self.name, self.step, self.count = handle, name, step, 0


class Op:
    __slots__ = ("eng", "fn", "waits", "inc", "sem", "semval", "is_dma", "line")

    def __init__(self, eng, fn, sem, is_dma):
        self.eng, self.fn, self.sem, self.is_dma = eng, fn, sem, is_dma
        self.waits = []
        self.inc = is_dma
        self.semval = None
        import sys as _s
        f = _s._getframe(3)
        self.line = (f.f_lineno, f.f_back.f_lineno if f.f_back else 0)


class PseudoDep:
    def __init__(self, sem, val):
        self.sem, self.semval = sem, val


class Plan:
    def __init__(self, nc):
        self.nc = nc
        self.stack = contextlib.ExitStack()
        self.ops = {e: [] for e in ISSUERS}
        self.dma_sems = []
        self.esem = {e: self.new_sem("e_" + e, 1) for e in COMPUTE}
        self.last_w = {}
        self.readers = {}
        self.names = None

    def new_sem(self, name, step=16):
        h = self.stack.enter_context(self.nc.semaphore(name))
        s = Sem(h, name, step)
        if step == 16:
            self.dma_sems.append(s)
        return s

    def sbuf(self, name, shape, dtype):
        return self.stack.enter_context(self.nc.sbuf_tensor(name, list(shape), dtype))

    def psum(self, name, shape, dtype):
        return self.stack.enter_context(self.nc.psum_tensor(name, list(shape), dtype))

    def _add(self, op, reads, writes):
        deps = {}
        for t in reads:
            d = self.last_w.get(t)
            if d is not None:
                deps[id(d)] = (d, True)
        for t in writes:
            d = self.last_w.get(t)
            if d is not None and id(d) not in deps:
                deps[id(d)] = (d, False)
            for r in self.readers.get(t, {}).values():
                if id(r) not in deps:
                    deps[id(r)] = (r, False)
        for d, raw in deps.values():
            if d is op:
                continue
            need = True
            if (not d.is_dma) and (not op.is_dma) and d.eng == op.eng:
                need = raw and op.eng != "pe"
            if need:
                if d.is_dma:
                    op.waits.append(PseudoDep(d.sem, d.sem.count))
                else:
                    d.inc = True
                    op.waits.append(d)
        for t in writes:
            self.last_w[t] = op
            self.readers[t] = {}
        for t in reads:
            key = op.sem.name if op.is_dma else op.eng
            self.readers.setdefault(t, {})[key] = op
        self.ops[op.eng].append(op)
        return op

    def c(self, eng, fn, reads=(), writes=()):
        return self._add(Op(eng, fn, self.esem[eng], False), reads, writes)

    def dma(self, eng, fn, sem, reads=(), writes=()):
        op = Op(eng, fn, sem, True)
        op.semval = sem.count + 16
        self._add(op, reads, writes)
        sem.count += 16
        return op

    def barrier(self):
        lasts = {}
        for e in COMPUTE:
            for op in reversed(self.ops[e]):
                if not op.is_dma and op.fn is not None:
                    op.inc = True
                    lasts[e] = op
                    break
        dmas = [PseudoDep(s, s.count) for s in self.dma_sems if s.count > 0]
        for q in ISSUERS:
            w = Op(q, None, None, False)
            w.waits = list(lasts.values()) + dmas
            self.ops[q].append(w)
        self.last_w = {}
        self.readers = {}

    def emit(self):
        nc = self.nc
        for e in COMPUTE:
            cnt = 0
            for op in self.ops[e]:
                if op.fn is not None and op.inc and not op.is_dma:
                    cnt += 1
                    op.semval = cnt
        final = [(s, s.count) for s in self.dma_sems if s.count > 0]
        plan = self

        def replay(engname, eh, tail=False):
            waited = {}
            for op in plan.ops[engname]:
                for d in op.waits:
                    v = d.semval
                    assert v is not None
                    if waited.get(d.sem.name, 0) < v:
                        eh.wait_ge(d.sem.handle, v)
                        waited[d.sem.name] = v
                if op.fn is None:
                    continue
                ins = op.fn(eh)
                if plan.names is not None:
                    plan.names[ins.ins.name] = (engname, op.line)
                if op.inc:
                    ins.then_inc(op.sem.handle, op.sem.step)
            if tail:
                for s, v in final:
                    if waited.get(s.name, 0) < v:
                        eh.wait_ge(s.handle, v)

        with nc.Block() as block:
            @block.tensor
            def _(pe):
                replay("pe", pe)

            @block.vector
            def _(dve):
                replay("dve", dve)

            @block.scalar
            def _(act):
                replay("act", act)

            @block.gpsimd
            def _(pool):
                replay("pool", pool)

            @block.sync
            def _(sp):
                replay("sp", sp, tail=True)


class Arena:
    def __init__(self, p, nbytes):
        self.t = p.sbuf("arena", [128, nbytes], U8)
        self.cap = nbytes
        self.off = 0
        self.hw = 0

    def reset(self, to=0):
        self.off = to

    def take(self, shape, dtype, parts=128):
        shape = list(shape)
        n = int(np.prod(shape)) * DSZ[dtype]
        n_al = (n + 63) // 64 * 64
        assert self.off + n_al <= self.cap, ("arena overflow", self.off, n_al, self.cap)
        ap = self.t[0:parts, self.off:self.off + n].bitcast(dtype)
        self.off += n_al
        self.hw = max(self.hw, self.off)
        if len(shape) > 1:
            names = [f"d{i}" for i in range(len(shape))]
            kw = {nm: sz for nm, sz in zip(names[:-1], shape[:-1])}
            ap = ap.rearrange("p (" + " ".join(names) + ") -> p " + " ".join(names), **kw)
        return ap


def build_program(dbg=None, stop_after=None):
    dbg = dbg or set()
    nc = bass.Bass("TRN2", target_bir_lowering=False)
    p = Plan(nc)

    def din(name, shape, dt=F32):
        return nc.dram_tensor(name, list(shape), dt, kind="ExternalInput").ap()

    def dscr(name, shape, dt):
        kind = "ExternalOutput" if name in dbg else "Internal"
        return nc.dram_tensor(name, list(shape), dt, kind=kind).ap()

    x_in = din("x", [S, D])
    W = {}
    for l in (0, 1):
        pre = f"l{l}_"
        if l == 0:
            W[pre + "w_in"] = din(pre + "w_in", [D, 1728])
            W[pre + "q_norm_g"] = din(pre + "q_norm_g", [384])
            W[pre + "w_q_up"] = din(pre + "w_q_up", [384, 768])
            W[pre + "kv_norm_g"] = din(pre + "kv_norm_g", [256])
            W[pre + "w_kv_up"] = din(pre + "w_kv_up", [256, 1024])
            W[pre + "conv_w"] = din(pre + "conv_w", [31, 512])
            for n in ("conv_b", "conv_ln_g", "conv_ln_b"):
                W[pre + n] = din(pre + n, [512])
        else:
            W[pre + "w_qkv"] = din(pre + "w_qkv", [D, 3072])
            for n in ("lambda_q1", "lambda_k1", "lambda_q2", "lambda_k2"):
                W[pre + n] = din(pre + n, [128])
            W[pre + "subln_g"] = din(pre + "subln_g", [256])
        W[pre + "w_o"] = din(pre + "w_o", [D, D])
        for n in ("ln1_g", "ln1_b", "ln2_g", "ln2_b"):
            W[pre + n] = din(pre + n, [D])
        W[pre + "router_w"] = din(pre + "router_w", [D, E])
        W[pre + "router_b"] = din(pre + "router_b", [E])
        W[pre + "w_gu"] = din(pre + "w_gu", [E, D, 2 * D])
        W[pre + "b_gu"] = din(pre + "b_gu", [E, 2 * D])
        W[pre + "w_dn"] = din(pre + "w_dn", [E, D, D])
        W[pre + "b_dn"] = din(pre + "b_dn", [E, D])
    c_ident = din("c_ident", [128, 128])
    c_ltri = din("c_ltri", [128, 128])
    c_mask = din("c_mask", [128, 4, 512])
    c_cos64 = din("c_cos64", [64, S])
    c_sin64 = din("c_sin64", [64, S])
    c_cos128 = din("c_cos128", [128, S])
    c_sin128 = din("c_sin128", [128, S])
    c_iota = din("c_iota", [128, 3, 32])
    c_tokid = din("c_tokid", [128, NT, 4], I32)
    c_zero = din("c_zero", [128, E * NJMAX], I32)
    out_ap = nc.dram_tensor("out", [S, D], F32, kind="ExternalOutput").ap()

    x1f = dscr("x1f", [S, D], F32)
    x1b = dscr("x1b", [S + 128, D], BF16)
    x2f = dscr("x2f", [S, D], F32)
    Ybuf = dscr("Ybuf", [4 * S + 128, D], F32)
    tokidx = dscr("tokidx", [128 * E * NJMAX, 1], I32)
    QnT = dscr("QnT", [4, 128, S], BF16)
    QrT = dscr("QrT", [4, 64, S], BF16)
    KnT = dscr("KnT", [4, 128, S], BF16)
    KrT = dscr("KrT", [64, S], BF16)
    Vx0 = dscr("Vx0", [4, NT, 128, 129], BF16)
    convT = dscr("convT", [4, 128, S], BF16)
    QT1 = dscr("QT1", [8, 128, S], BF16)
    KT1 = dscr("KT1", [8, 128, S], BF16)
    Vx1 = dscr("Vx1", [4, NT, 128, 257], BF16)
    attnD = dscr("attnD", [8, 128, S], BF16)
    dbgD = dscr("dbgD", [128, 4096], F32)

    A = Arena(p, 204 * 1024)
    pb = [p.psum(f"pb{i}", [128, 512], F32)[:] for i in range(8)]
    pbn = [f"pb{i}" for i in range(8)]

    def MM(out, lhsT, rhs, start, stop, r, w, skip=False):
        p.c("pe", lambda e: e.matmul(out, lhsT, rhs, start=start, stop=stop, skip_group_check=skip), r, w)

    def TR(out, in_, ident, r, w):
        p.c("pe", lambda e: e.transpose(out, in_, ident), r, w)

    def TS(eng, out, in0, s1, s2, op0, op1, r, w):
        if op1 is None:
            p.c(eng, lambda e: e.tensor_scalar(out, in0, s1, None, op0), r, w)
        else:
            p.c(eng, lambda e: e.tensor_scalar(out, in0, s1, s2, op0, op1), r, w)

    def TT(eng, out, in0, in1, op, r, w):
        p.c(eng, lambda e: e.tensor_tensor(out, in0, in1, op), r, w)

    def STT(eng, out, in0, sc, in1, op0, op1, r, w):
        p.c(eng, lambda e: e.scalar_tensor_tensor(out, in0, sc, in1, op0, op1), r, w)

    def ACT(out, in_, func, r, w, bias=None, scale=1.0, accum=None):
        def fn(e):
            kw = {}
            if bias is not None:
                kw["bias"] = bias
            if accum is not None:
                kw["accum_out"] = accum
            return e.activation(out, in_, func, scale=scale, **kw)
        p.c("act", fn, r, w)

    def RSQRT(out, in_, scale, eps, r, w):
        ACT(out, in_, AF.Sqrt, r, w, bias=epsT[:, 0:1] if eps == LN_EPS else epsT[:, 1:2], scale=scale)
        p.c("dve", lambda e: e.reciprocal(out, out), w, w)

    def CP(eng, out, in_, r, w):
        if eng == "act":
            p.c("act", lambda e: e.copy(out, in_), r, w)
        else:
            p.c(eng, lambda e: e.tensor_copy(out, in_), r, w)

    def MS(eng, out, val, w):
        p.c(eng, lambda e: e.memset(out, val), (), w)

    def DMA(q, out, in_, sem, r, w, slow=False):
        if q == "pool":
            sem = SEM(sem.name[2:] + "_sw")
        if slow:
            p.dma(q, lambda e: e.dma_start(out=out, in_=in_, allow_slow_non_contiguous=True), sem, r, w)
        else:
            p.dma(q, lambda e: e.dma_start(out=out, in_=in_), sem, r, w)

    def GATHER(out, src, idx, sem, r, w):
        nrows = src.shape[0]
        p.dma("pool", lambda e: e.indirect_dma_start(
            out=out, out_offset=None, in_=src,
            in_offset=bass.IndirectOffsetOnAxis(ap=idx, axis=0)), sem, r, w)

    def SCATTER(dst, src, idx, sem, r, w):
        nrows = dst.shape[0]
        p.dma("pool", lambda e: e.indirect_dma_start(
            out=dst, out_offset=bass.IndirectOffsetOnAxis(ap=idx, axis=0),
            in_=src, in_offset=None), sem, r, w)

    sem_pool = {}

    def SEM(name):
        if name.startswith("d_"):
            name = name[2:]
        if name not in sem_pool:
            sem_pool[name] = p.new_sem("d_" + name)
        return sem_pool[name]

    bank_rr = [0]

    def nbank(lo=0, hi=7):
        b = lo + bank_rr[0] % (hi - lo)
        bank_rr[0] += 1
        return b

    def colvec(dst, src_vec, n, sem, wtok):
        DMA("sp", dst, src_vec.rearrange("(c p) -> p c", p=128), sem, (), [wtok], slow=True)

    identF = A.take([128], F32)
    identB = A.take([128], BF16)
    onesF = A.take([128], F32)
    onesB = A.take([128], BF16)
    ltriB = A.take([128], BF16)
    maskB = A.take([4, 512], BF16)
    iotaF = A.take([3, 32], F32)
    tokid = A.take([NT, 4], I32)
    s_c = SEM("const")
    DMA("sp", identF, c_ident, s_c, (), ["identF"])
    DMA("pool", identB, c_ident, s_c, (), ["identB"])
    DMA("pool", ltriB, c_ltri, s_c, (), ["ltriB"])
    DMA("pool", maskB, c_mask, s_c, (), ["maskB"])
    DMA("sp", iotaF, c_iota, s_c, (), ["iotaF"])
    DMA("sp", tokid, c_tokid, s_c, (), ["tokid"])
    epsT = A.take([2], F32)
    MS("dve", epsT[:, 0:1], LN_EPS, ["epsT"])
    MS("dve", epsT[:, 1:2], RMS_EPS, ["epsT"])
    MS("dve", onesF, 1.0, ["onesF"])
    MS("dve", onesB, 1.0, ["onesB"])
    negB = maskB
    TS("dve", negB, maskB, 1.0, 30000.0, ALU.subtract, ALU.mult, ["maskB"], ["maskB", "negB"])
    KEEP = A.off
    zrow = A.take([D], BF16)
    MS("dve", zrow, 0.0, ["zrow"])
    DMA("sp", x1b[S:S + 128, :], zrow, s_c, ["zrow"], ())
    p.barrier()
    route_holder = []

    def build_xT_block(src, b, xTb, xin_bufs, tagp):
        def load(bb):
            for ti in range(4):
                t = 4 * bb + ti
                xn = f"{tagp}xin{ti}"
                DMA("sp", xin_bufs[ti], src[t * 128:(t + 1) * 128, :], SEM(xn), (), [xn])
        if b == 0:
            load(0)
        for ti in range(4):
            xin = xin_bufs[ti]
            xn = f"{tagp}xin{ti}"
            for half in range(2):
                bk = nbank()
                for q in range(4):
                    kc = half * 4 + q
                    TR(pb[bk][:, q * 128:(q + 1) * 128], xin[:, kc * 128:(kc + 1) * 128], identF,
                       [xn, "identF"], [pbn[bk]])
                CP("act" if half == 0 else "dve",
                   xTb[:, half * 4:half * 4 + 4, ti * 128:(ti + 1) * 128],
                   pb[bk].rearrange("p (q c) -> p q c", q=4), [pbn[bk]], [f"{tagp}xTb"])
        if b + 1 < NB:
            load(b + 1)

    def phase_A0(src):
        A.reset(KEEP2)
        w_in = A.take([8, 1728], BF16)
        wkrs = A.take([8, 64], BF16)
        wq = A.take([3, 768], BF16)
        wqs = A.take([3, 4, 64], BF16)
        wkv = A.take([2, 1024], BF16)
        gq = A.take([3], F32)
        gkv = A.take([2], F32)
        cvec = A.take([3, 4], F32)
        cwrow = A.take([512], F32, parts=31)
        cw = A.take([4, 31], F32)
        cos = A.take([S], F32, parts=64)
        sin = A.take([S], F32, parts=64)
        s_w = SEM("wA")
        win_v = W["l0_w_in"].rearrange("(kc p) n -> p kc n", p=128)
        for kc in range(8):
            DMA("pool", w_in[:, kc, 0:864], win_v[:, kc, 0:864], s_w, (), ["w_in"])
            DMA("pool", w_in[:, kc, 864:1728], win_v[:, kc, 864:1728], s_w, (), ["w_in"])
            DMA("pool", wkrs[:, kc, 0:32], win_v[:, kc, 672:704], s_w, (), ["wkrs"])
            DMA("pool", wkrs[:, kc, 32:64], win_v[:, kc, 640:672], s_w, (), ["wkrs"])
        wq_v = W["l0_w_q_up"].rearrange("(kc p) n -> p kc n", p=128)
        for kc in range(3):
            DMA("pool", wq[:, kc, :], wq_v[:, kc, :], s_w, (), ["wq"])
            for h in range(4):
                c0 = h * 192 + 128
                DMA("pool", wqs[:, kc, h, 0:32], wq_v[:, kc, c0 + 32:c0 + 64], s_w, (), ["wqs"])
                DMA("pool", wqs[:, kc, h, 32:64], wq_v[:, kc, c0:c0 + 32], s_w, (), ["wqs"])
        wkv_v = W["l0_w_kv_up"].rearrange("(kc p) n -> p kc n", p=128)
        for kc in range(2):
            DMA("pool", wkv[:, kc, :], wkv_v[:, kc, :], s_w, (), ["wkv"])
        colvec(gq, W["l0_q_norm_g"], 3, s_w, "gq")
        colvec(gkv, W["l0_kv_norm_g"], 2, s_w, "gkv")
        colvec(cvec[:, 0, :], W["l0_conv_b"], 4, s_w, "cvec")
        colvec(cvec[:, 1, :], W["l0_conv_ln_g"], 4, s_w, "cvec")
        colvec(cvec[:, 2, :], W["l0_conv_ln_b"], 4, s_w, "cvec")
        DMA("sp", cwrow, W["l0_conv_w"], s_w, (), ["cwrow"])
        DMA("sp", cos, c_cos64, s_w, (), ["cos"])
        DMA("sp", sin, c_sin64, s_w, (), ["sin"])
        for cc in range(4):
            bk = nbank()
            TR(pb[bk][:, 0:31], cwrow[:, cc * 128:(cc + 1) * 128], identF[0:31, 0:31], ["cwrow", "identF"], [pbn[bk]])
            CP("dve", cw[:, cc, :], pb[bk][:, 0:31], [pbn[bk]], ["cw"])

        dg = A.take([4, 31, 128], BF16)
        for cc in range(4):
            for j in range(31):
                TS("dve", dg[:, cc, j, :], identF, cw[:, cc, j:j + 1], None, ALU.mult, None, ["identF", "cw"], ["dg"])
        xin_bufs = [A.take([D], F32) for _ in range(4)]
        xTb = A.take([8, 512], BF16)
        ubuf = A.take([4, 542], BF16)
        ybuf = A.take([4, 512], F32)
        t512 = [A.take([512], F32) for _ in range(6)]
        qlT = A.take([3, 512], BF16)
        kvlT = A.take([2, 512], BF16)
        rstdq = A.take([512], F32)
        rstdkv = A.take([512], F32)
        rstdkv_tok = A.take([4], F32)
        mean_t = A.take([512], F32)
        rstd_t = A.take([512], F32)
        ob = [A.take([512], BF16) for _ in range(4)]
        vxt = [A.take([4, 129], BF16) for _ in range(2)]
        MS("dve", ubuf, 0.0, ["ubuf0", "ubuf1", "ubuf2", "ubuf3"])
        for i in range(2):
            MS("dve", vxt[i], 1.0, [f"vxt{i}"])
        obi = [0]

        def out_store(dst, src_tile_fn, parts=128):
            k = obi[0] % 4
            obi[0] += 1
            tn = f"ob{k}"
            src_tile_fn(ob[k][0:parts, :], tn)
            DMA("sp", dst, ob[k][0:parts, :], SEM(tn), [tn], ())

        for b in range(NB):
            t0 = b * 512
            build_xT_block(src, b, xTb, xin_bufs, "A")
            xr = ["AxTb"]

            def proj(col0, m, wt=w_in, wn="w_in"):
                bk = nbank()
                for kc in range(8):
                    MM(pb[bk][0:m, :], wt[:, kc, col0:col0 + m], xTb[:, kc, :], kc == 0, kc == 7,
                       xr + [wn], [pbn[bk]])
                return bk

            for cc in range(4):
                un = f"ubuf{cc}"
                ba = proj(704 + cc * 128, 128)
                bg = proj(1216 + cc * 128, 128)
                sg = t512[0]
                ACT(sg, pb[bg], AF.Sigmoid, [pbn[bg]], ["t0"])
                TT("dve", ubuf[:, cc, 30:542], pb[ba], sg, ALU.mult, [pbn[ba], "t0"], [un])
                yn = f"y{cc}"
                bc = nbank()
                for j in range(31):
                    MM(pb[bc], dg[:, cc, j, :], ubuf[:, cc, j:j + 512], j == 0, j == 30, ["dg", un], [pbn[bc]])
                ACT(ybuf[:, cc, :], pb[bc], AF.Identity, [pbn[bc], "cvec"], [yn], bias=cvec[:, 0, cc:cc + 1])
                CP("pool", ubuf[:, cc, 0:30], ubuf[:, cc, 512:542], [un], [un])
            def lat(col0, nch, lT, lTn, g, gn, rstd, rstdn, dim):
                bss = nbank()
                for mc in range(nch):
                    bk = proj(col0 + mc * 128, 128)
                    ACT(lT[:, mc, :], pb[bk], AF.Identity, [pbn[bk], gn], [lTn], scale=g[:, mc:mc + 1])
                    sq = t512[mc % 2]
                    sqn = f"t{mc % 2}"
                    TT("dve", sq, pb[bk], pb[bk], ALU.mult, [pbn[bk]], [sqn]) if False else \
                        ACT(sq, pb[bk], AF.Square, [pbn[bk]], [sqn])
                    MM(pb[bss], onesF, sq, mc == 0, mc == nch - 1, ["onesF", sqn], [pbn[bss]])
                    if lTn == "kvlT":
                        for ti in range(4):
                            MM(pb[7][:, ti * 2:ti * 2 + 1], sq[:, ti * 128:(ti + 1) * 128], onesF[:, 0:1],
                               mc == 0 and ti == 0, mc == nch - 1 and ti == 3, [sqn, "onesF"], ["pb7"], skip=True)
                RSQRT(rstd, pb[bss], 1.0 / dim, RMS_EPS, [pbn[bss]], [rstdn])

            lat(0, 3, qlT, "qlT", gq, "gq", rstdq, "rstdq", 384)
            lat(384, 2, kvlT, "kvlT", gkv, "gkv", rstdkv, "rstdkv", 256)
            RSQRT(rstdkv_tok, pb[7].rearrange("p (a b) -> p a b", b=2)[:, 0:4, 0], 1.0 / 256, RMS_EPS, ["pb7"], ["rkt"])

            b1 = nbank()
            b2 = nbank()
            for cc in range(4):
                MM(pb[b1], onesF, ybuf[:, cc, :], cc == 0, cc == 3, ["onesF", f"y{cc}"], [pbn[b1]])
            for cc in range(4):
                sq = t512[1 + cc % 2]
                sqn = f"t{1 + cc % 2}"
                TT("pool", sq, ybuf[:, cc, :], ybuf[:, cc, :], ALU.mult, [f"y{cc}"], [sqn])
                MM(pb[b2], onesF, sq, cc == 0, cc == 3, ["onesF", sqn], [pbn[b2]])
            ACT(mean_t, pb[b1], AF.Identity, [pbn[b1]], ["mean_t"], scale=1.0 / 512)
            TT("pool", t512[3], mean_t, mean_t, ALU.mult, ["mean_t"], ["t3"])
            STT("dve", rstd_t, pb[b2], 1.0 / 512, t512[3], ALU.mult, ALU.subtract, [pbn[b2], "t3"], ["rstd_t"])
            RSQRT(rstd_t, rstd_t, 1.0, LN_EPS, ["rstd_t"], ["rstd_t"])
            for cc in range(4):
                TT("pool", t512[4], ybuf[:, cc, :], mean_t, ALU.subtract, [f"y{cc}", "mean_t"], ["t4"])
                TT("pool", t512[5], t512[4], rstd_t, ALU.mult, ["t4", "rstd_t"], ["t5"])

                def fill(o, tn, cc=cc):
                    ACT(o, t512[5], AF.Silu, ["t5", "cvec"], [tn], bias=cvec[:, 2, cc:cc + 1],
                        scale=cvec[:, 1, cc:cc + 1])
                out_store(convT[cc][:, t0:t0 + 512], fill)

            def rope_store(bm, bs, dst, rstd=None, rstdn=None):
                TT("dve", t512[0][0:64], pb[bm][0:64], cos[:, t0:t0 + 512], ALU.mult, [pbn[bm], "cos"], ["t0"])
                TT("dve", t512[1][0:64], pb[bs][0:64], sin[:, t0:t0 + 512], ALU.mult, [pbn[bs], "sin"], ["t1"])

                def fill(o, tn):
                    if rstd is None:
                        TT("pool", o, t512[0][0:64], t512[1][0:64], ALU.add, ["t0", "t1"], [tn])
                    else:
                        TT("pool", t512[2][0:64], t512[0][0:64], t512[1][0:64], ALU.add, ["t0", "t1"], ["t2"])
                        TT("pool", o, t512[2][0:64], rstd[0:64], ALU.mult, ["t2", rstdn], [tn])
                out_store(dst, fill, parts=64)

            bm = proj(640, 64)
            bs = proj(0, 64, wkrs, "wkrs")
            rope_store(bm, bs, KrT[:, t0:t0 + 512])

            for h in range(4):
                bk = nbank()
                for mc in range(3):
                    MM(pb[bk], wq[:, mc, h * 192:h * 192 + 128], qlT[:, mc, :], mc == 0, mc == 2,
                       ["wq", "qlT"], [pbn[bk]])

                def fill(o, tn, bk=bk):
                    TT("dve", o, pb[bk], rstdq, ALU.mult, [pbn[bk], "rstdq"], [tn])
                out_store(QnT[h][:, t0:t0 + 512], fill)
                bm = nbank()
                bs = nbank()
                for mc in range(3):
                    MM(pb[bm][0:64], wq[:, mc, h * 192 + 128:h * 192 + 192], qlT[:, mc, :], mc == 0, mc == 2,
                       ["wq", "qlT"], [pbn[bm]])
                for mc in range(3):
                    MM(pb[bs][0:64], wqs[:, mc, h, :], qlT[:, mc, :], mc == 0, mc == 2, ["wqs", "qlT"], [pbn[bs]])
                rope_store(bm, bs, QrT[h][:, t0:t0 + 512], rstdq, "rstdq")

            for h in range(4):
                bk = nbank()
                for mc in range(2):
                    MM(pb[bk], wkv[:, mc, h * 256:h * 256 + 128], kvlT[:, mc, :], mc == 0, mc == 1,
                       ["wkv", "kvlT"], [pbn[bk]])

                def fill(o, tn, bk=bk):
                    TT("dve", o, pb[bk], rstdkv, ALU.mult, [pbn[bk], "rstdkv"], [tn])
                out_store(KnT[h][:, t0:t0 + 512], fill)
            for ti in range(4):
                t = 4 * b + ti
                bk = nbank()
                for h in range(4):
                    for mc in range(2):
                        MM(pb[bk][:, h * 128:(h + 1) * 128], kvlT[:, mc, ti * 128:(ti + 1) * 128],
                           wkv[:, mc, h * 256 + 128:h * 256 + 256], mc == 0 and h == 0, mc == 1 and h == 3,
                           ["kvlT", "wkv"], [pbn[bk]], skip=True)
                vt = vxt[t % 2]
                vn = f"vxt{t % 2}"
                TS("dve", vt[:, :, 0:128], pb[bk].rearrange("p (h c) -> p h c", h=4), rstdkv_tok[:, ti:ti + 1],
                   None, ALU.mult, None, [pbn[bk], "rkt"], [vn])
                DMA("sp", Vx0[:, t].rearrange("h p c -> p h c"), vt, SEM(vn), [vn], ())
        p.barrier()

    def attn_phase(nheads, maps_of_head, dv, scale, load_head, post, attnT, consts_extra=None):
        pT = [A.take([512], BF16) for _ in range(3)]
        for h in range(nheads):
            maps, vx, vn = load_head(h)
            steps = []
            for j in range(NB):
                for mi, parts in enumerate(maps):
                    for i in range(4 * j + 4):
                        steps.append((j, mi, parts, i))

            def emit_scores(n):
                j, mi, parts, i = steps[n]
                sb = 4 + n % 3
                diag = i - 4 * j >= 0
                c0 = 128 * max(i - 4 * j, 0)
                for ci, (qa, ka, qn, kn) in enumerate(parts):
                    MM(pb[sb][:, c0:512], ka[:, i * 128:(i + 1) * 128], qa[:, j * 512 + c0:(j + 1) * 512],
                       ci == 0, ci == len(parts) - 1 and not diag, [qn, kn], [pbn[sb]])
                if diag:
                    MM(pb[sb][:, c0:512], identB, negB[:, i - 4 * j, c0:512], False, True, ["identB", "negB"], [pbn[sb]])

            pending = []
            pend_at = [0]
            emit_scores(0)
            emit_scores(1)
            for n, (j, mi, parts, i) in enumerate(steps):
                if n + 2 < len(steps):
                    emit_scores(n + 2)
                sb = 4 + n % 3
                pt = pT[n % 3]
                ptn = f"pT{n % 3}"
                r = i - 4 * j
                c0 = 128 * max(r, 0)
                ACT(pt[:, c0:512], pb[sb][:, c0:512], AF.Exp, [pbn[sb]], [ptn], scale=scale)
                for qs in range(4):
                    if r >= 0 and qs < r:
                        continue
                    MM(pb[qs][:, 0:dv + 1], pt[:, qs * 128:(qs + 1) * 128], vx[:, i, :],
                       i == 0, i == 4 * j + qs, [ptn, vn], [pbn[qs]])
                if pending and n - pend_at[0] >= 6:
                    for fn in pending:
                        fn()
                    pending.clear()
                if i == 4 * j + 3:
                    for fn in pending:
                        fn()
                    pending.clear()
                    pending.extend(post(h, mi, j) or [])
                    pend_at[0] = n
            for fn in pending:
                fn()
            pending.clear()

    def phase_B0(attnT):
        qn_b = [A.take([S], BF16) for _ in range(2)]
        kn_b = [A.take([S], BF16) for _ in range(2)]
        qr_b = [A.take([S], BF16, parts=64) for _ in range(2)]
        kr = A.take([S], BF16, parts=64)
        vx_b = [A.take([NT, 129], BF16) for _ in range(2)]
        rinv = A.take([4], F32)
        on = [A.take([128], BF16) for _ in range(4)]
        DMA("sp", kr, KrT, SEM("kr"), (), ["kr"])
        oi = [0]

        def load_head(h):
            s = h % 2
            sm = SEM(f"hd{s}")
            DMA("sp", qn_b[s], QnT[h], sm, (), [f"qn{s}"])
            DMA("sp", kn_b[s], KnT[h], sm, (), [f"kn{s}"])
            DMA("sp", qr_b[s], QrT[h], sm, (), [f"qr{s}"])
            DMA("sp", vx_b[s], Vx0[h].rearrange("t p c -> p t c"), sm, (), [f"vx{s}"])
            maps = [[(qn_b[s], kn_b[s], f"qn{s}", f"kn{s}"), (qr_b[s], kr, f"qr{s}", "kr")]]
            return maps, vx_b[s], f"vx{s}"

        def post(h, mi, j):
            for qs in range(4):
                p.c("dve", lambda e, qs=qs: e.reciprocal(rinv[:, qs:qs + 1], pb[qs][:, 128:129]), [pbn[qs]], ["rinv"])
                TS("dve", on[qs], pb[qs][:, 0:128], rinv[:, qs:qs + 1], None, ALU.mult, None, [pbn[qs], "rinv"], [f"on{qs}"])

            def later():
                pv = pb[7].bitcast(BF16)
                for qs in range(4):
                    TR(pv[:, qs * 128:(qs + 1) * 128], on[qs], identB, [f"on{qs}", "identB"], ["pb7"])
                CP("dve", attnT[:, h, j * 512:(j + 1) * 512], pv[:, 0:512], ["pb7"], ["attnT"])
            return [later]

        attn_phase(4, None, 128, 192.0 ** -0.5, load_head, post, attnT)

    def phase_A1(src):
        A.reset(KEEP2)
        wqkv = A.take([8, 3072], BF16)
        wsw = A.take([8, 16, 128], BF16)
        cos = A.take([S], F32)
        sin = A.take([S], F32)
        s_w = SEM("wA")
        wv = W["l1_w_qkv"].rearrange("(kc p) n -> p kc n", p=128)
        for kc in range(8):
            for c3 in range(3):
                DMA("pool", wqkv[:, kc, c3 * 1024:(c3 + 1) * 1024], wv[:, kc, c3 * 1024:(c3 + 1) * 1024], s_w, (), ["wqkv"])
            sv = wv[:, kc, 0:2048].rearrange("p (hb two c) -> p hb two c", two=2, c=64)
            DMA("pool", wsw[:, kc, :, 0:64], sv[:, :, 1, :], s_w, (), ["wsw"])
            DMA("pool", wsw[:, kc, :, 64:128], sv[:, :, 0, :], s_w, (), ["wsw"])
        DMA("sp", cos, c_cos128, s_w, (), ["cos"])
        DMA("sp", sin, c_sin128, s_w, (), ["sin"])
        xin_bufs = [A.take([D], F32) for _ in range(4)]
        xTb = A.take([8, 512], BF16)
        t1 = [A.take([512], F32) for _ in range(2)]
        t2 = [A.take([512], F32) for _ in range(2)]
        ob = [A.take([512], BF16) for _ in range(4)]
        vxt = [A.take([4, 257], BF16) for _ in range(2)]
        for i in range(2):
            MS("dve", vxt[i], 1.0, [f"vxt{i}"])
        cnt = [0]
        for b in range(NB):
            t0 = b * 512
            build_xT_block(src, b, xTb, xin_bufs, "A")
            for hb in range(16):
                bm = nbank()
                bs = nbank()
                for kc in range(8):
                    MM(pb[bm], wqkv[:, kc, hb * 128:(hb + 1) * 128], xTb[:, kc, :], kc == 0, kc == 7, ["wqkv", "AxTb"], [pbn[bm]])
                for kc in range(8):
                    MM(pb[bs], wsw[:, kc, hb, :], xTb[:, kc, :], kc == 0, kc == 7, ["wsw", "AxTb"], [pbn[bs]])
                k = cnt[0] % 2
                o = cnt[0] % 4
                cnt[0] += 1
                TT("dve", t1[k], pb[bm], cos[:, t0:t0 + 512], ALU.mult, [pbn[bm], "cos"], [f"t1{k}"])
                TT("dve", t2[k], pb[bs], sin[:, t0:t0 + 512], ALU.mult, [pbn[bs], "sin"], [f"t2{k}"])
                TT("pool", ob[o], t1[k], t2[k], ALU.add, [f"t1{k}", f"t2{k}"], [f"ob{o}"])
                dst = QT1[hb] if hb < 8 else KT1[hb - 8]
                DMA("sp", dst[:, t0:t0 + 512], ob[o], SEM(f"ob{o}"), [f"ob{o}"], ())
            for ti in range(4):
                t = 4 * b + ti
                vt = vxt[t % 2]
                vn = f"vxt{t % 2}"
                for half in range(2):
                    bk = nbank()
                    for kc in range(8):
                        MM(pb[bk], xTb[:, kc, ti * 128:(ti + 1) * 128], wqkv[:, kc, 2048 + half * 512:2048 + (half + 1) * 512],
                           kc == 0, kc == 7, ["AxTb", "wqkv"], [pbn[bk]])
                    CP("act", vt[:, 2 * half:2 * half + 2, 0:256], pb[bk].rearrange("p (h c) -> p h c", h=2), [pbn[bk]], [vn])
                DMA("sp", Vx1[:, t].rearrange("h p c -> p h c"), vt, SEM(vn), [vn], ())
        p.barrier()

    def phase_B1(attnT):
        li = 0.8 - 0.6 * math.exp(-0.3)
        q_b = [[A.take([S], BF16) for _ in range(2)] for _ in range(2)]
        k_b = [[A.take([S], BF16) for _ in range(2)] for _ in range(2)]
        vx_b = [A.take([NT, 257], BF16) for _ in range(2)]
        lam4 = A.take([4, 128], F32)
        lp = A.take([2, 128], F32)
        ls = A.take([2], F32)
        neglam = A.take([1], F32)
        gsub = A.take([256], F32)
        o1 = A.take([4, 256], F32)
        o2 = A.take([4, 256], F32)
        dd = A.take([256], F32)
        sqj = A.take([256], F32)
        ss = A.take([1], F32)
        rinv = A.take([4], F32)
        rs = A.take([1], F32)
        on = [A.take([256], BF16) for _ in range(4)]
        s_w = SEM("wB")
        for i, n in enumerate(("lambda_q1", "lambda_k1", "lambda_q2", "lambda_k2")):
            DMA("sp", lam4[:, i, :], W["l1_" + n].partition_broadcast(128), s_w, (), ["lam4"])
        DMA("sp", gsub, W["l1_subln_g"].partition_broadcast(128), s_w, (), ["gsub"])
        for m in range(2):
            TT("dve", lp[:, m, :], lam4[:, 2 * m, :], lam4[:, 2 * m + 1, :], ALU.mult, ["lam4"], ["lp"])
        p.c("dve", lambda e: e.tensor_reduce(ls, lp, AX.X, ALU.add), ["lp"], ["ls"])
        ACT(ls, ls, AF.Exp, ["ls"], ["ls"])
        TT("dve", neglam, ls[:, 1:2], ls[:, 0:1], ALU.subtract, ["ls"], ["neglam"])
        TS("dve", neglam, neglam, -li, None, ALU.add, None, ["neglam"], ["neglam"])
        TS("dve", gsub, gsub, 1.0 - li, None, ALU.mult, None, ["gsub"], ["gsub"])
        oi = [0]

        def load_head(h):
            s = h % 2
            sm = SEM(f"hd{s}")
            maps = []
            for m in range(2):
                DMA("sp", q_b[s][m], QT1[2 * h + m], sm, (), [f"q{s}{m}"])
                DMA("sp", k_b[s][m], KT1[2 * h + m], sm, (), [f"k{s}{m}"])
                maps.append([(q_b[s][m], k_b[s][m], f"q{s}{m}", f"k{s}{m}")])
            DMA("sp", vx_b[s], Vx1[h].rearrange("t p c -> p t c"), sm, (), [f"vx{s}"])
            return maps, vx_b[s], f"vx{s}"

        def post(h, mi, j):
            dst = o1 if mi == 0 else o2
            dn_ = "o1" if mi == 0 else "o2"
            for qs in range(4):
                p.c("dve", lambda e, qs=qs: e.reciprocal(rinv[:, qs:qs + 1], pb[qs][:, 256:257]), [pbn[qs]], ["rinv"])
                TS("dve", dst[:, qs, :], pb[qs][:, 0:256], rinv[:, qs:qs + 1], None, ALU.mult, None, [pbn[qs], "rinv"], [f"{dn_}{qs}"])
            if mi == 0:
                return []
            for qs in range(4):
                STT("dve", dd, o2[:, qs, :], neglam, o1[:, qs, :], ALU.mult, ALU.add, [f"o2{qs}", "neglam", f"o1{qs}"], ["dd"])
                ACT(sqj, dd, AF.Square, ["dd"], ["sqj", "ss"], accum=ss)
                RSQRT(rs, ss, 1.0 / 256, RMS_EPS, ["ss"], ["rs"])
                STT("dve", on[qs], dd, rs, gsub, ALU.mult, ALU.mult, ["dd", "rs", "gsub"], [f"on{qs}"])

            def later():
                for qs in range(4):
                    t = 4 * j + qs
                    pv = pb[7].bitcast(BF16)
                    for c in range(2):
                        TR(pv[:, c * 128:(c + 1) * 128], on[qs][:, c * 128:(c + 1) * 128], identB, [f"on{qs}", "identB"], ["pb7"])
                    CP("dve", attnT[:, 2 * h:2 * h + 2, t * 128:(t + 1) * 128],
                       pv[:, 0:256].rearrange("p (c q) -> p c q", c=2), ["pb7"], ["attnT"])
            return [later]

        attn_phase(4, None, 256, 128.0 ** -0.5, load_head, post, attnT)

    def phase_C(l, attnT, nattn, conv_src, xres, route):
        pre = f"l{l}_"
        wo = A.take([8, D], BF16)
        s_w = SEM("wC")
        wo_v = W[pre + "w_o"].rearrange("(kc p) n -> p kc n", p=128)
        for kc in range(8):
            DMA("pool", wo[:, kc, :], wo_v[:, kc, :], s_w, (), ["wo"])
        cat = [(attnT[:, c, :], "attnT") for c in range(nattn)]
        if conv_src is not None:
            cvT = A.take([4, S], BF16)
            for cc in range(4):
                DMA("sp", cvT[:, cc, :], conv_src[cc], s_w, (), ["cvT"])
            cat += [(cvT[:, cc, :], "cvT") for cc in range(4)]
        gB = A.take([D], F32)
        bB = A.take([D], F32)
        DMA("sp", gB, W[pre + "ln1_g"].partition_broadcast(128), s_w, (), ["gB"])
        DMA("sp", bB, W[pre + "ln1_b"].partition_broadcast(128), s_w, (), ["bB"])
        rw = A.take([8, E], F32)
        DMA("sp", rw, W[pre + "router_w"].rearrange("(kc p) n -> p kc n", p=128), s_w, (), ["rw"])
        rbB = A.take([E], F32)
        DMA("sp", rbB, W[pre + "router_b"].partition_broadcast(128), s_w, (), ["rbB"])
        xin = [A.take([D], F32) for _ in range(2)]
        hb = [A.take([D], F32) for _ in range(2)]
        x1t = [A.take([D], F32) for _ in range(2)]
        x1bt = [A.take([D], BF16) for _ in range(2)]
        x1T = A.take([8, 128], F32)
        stats = A.take([2, 6], F32)
        mv = A.take([2], F32)
        rstd = A.take([1], F32)
        prev = None
        for t in range(NT):
            s = t % 2
            DMA("sp", xin[s], xres[t * 128:(t + 1) * 128, :], SEM(f"Cxin{s}"), (), [f"xin{s}"])
            bks = [nbank(0, 5), nbank(0, 5)]
            for half in range(2):
                for c, (ca, cn) in enumerate(cat):
                    MM(pb[bks[half]], ca[:, t * 128:(t + 1) * 128], wo[:, c, half * 512:(half + 1) * 512],
                       c == 0, c == len(cat) - 1, [cn, "wo"], [pbn[bks[half]]])
            hn = f"hb{s}"
            for half in range(2):
                STT("dve", hb[s][:, half * 512:(half + 1) * 512], xin[s][:, half * 512:(half + 1) * 512], DN_ALPHA,
                    pb[bks[half]], ALU.mult, ALU.add, [f"xin{s}", pbn[bks[half]]], [hn])
            layer_norm_tile(hb[s], hn, x1t[s], f"x1t{s}", gB, bB, stats, mv, rstd)
            DMA("sp", x1f[t * 128:(t + 1) * 128, :], x1t[s], SEM(f"Cx1f{s}"), [f"x1t{s}"], ())
            CP("act", x1bt[s], x1t[s], [f"x1t{s}"], [f"x1bt{s}"])
            DMA("sp", x1b[t * 128:(t + 1) * 128, :], x1bt[s], SEM(f"Cx1b{s}"), [f"x1bt{s}"], ())
            for half in range(2):
                bk = nbank(0, 5)
                for q in range(4):
                    kc = half * 4 + q
                    TR(pb[bk][:, q * 128:(q + 1) * 128], x1t[s][:, kc * 128:(kc + 1) * 128], identF,
                       [f"x1t{s}", "identF"], [pbn[bk]])
                CP("act", x1T[:, half * 4:half * 4 + 4, :], pb[bk].rearrange("p (q c) -> p q c", q=4),
                   [pbn[bk]], ["x1T"])
            bk = 5 + t % 2
            for kc in range(8):
                MM(pb[bk][:, 0:E], x1T[:, kc, :], rw[:, kc, :], kc == 0, kc == 7, ["x1T", "rw"], [pbn[bk]])
            if prev is not None:
                route.tile(*prev)
            prev = (t, pb[bk][:, 0:E], pbn[bk], rbB)
        route.tile(*prev)
        p.barrier()

    def layer_norm_tile(h, hn, out, on, gB, bB, stats, mv, rstd, gb_eng="dve"):
        for c in range(2):
            p.c("dve", lambda e, c=c: e.bn_stats(stats[:, c, :], h[:, c * 512:(c + 1) * 512]), [hn], ["stats"])
        p.c("dve", lambda e: e.bn_aggr(mv, stats.rearrange("p a b -> p (a b)")), ["stats"], ["mv"])
        RSQRT(rstd, mv[:, 1:2], 1.0, LN_EPS, ["mv"], ["rstd"])
        TS("dve", out, h, mv[:, 0:1], rstd, ALU.subtract, ALU.mult, [hn, "mv", "rstd"], [on])
        TT(gb_eng, out, out, gB, ALU.mult, [on, "gB"], [on])
        TT(gb_eng, out, out, bB, ALU.add, [on, "bB"], [on])

    class Route:
        def __init__(self):
            self.gates = A.take([NT, 4], F32)
            self.eidx = A.take([NT, 4], F32)
            self.yrow = A.take([NT, 4], I32)
            self.cum = A.take([E], BF16)
            self.logit = A.take([E], F32)
            self.mx = A.take([8], F32)
            self.mi = A.take([8], U32)
            self.mask = A.take([E], BF16)
            self.ex = A.take([4], F32)
            self.sm = A.take([1], F32)
            self.rank = A.take([E], F32)
            self.oh = A.take([E], F32)
            self.rk = A.take([4], F32)
            self.ri = A.take([4], I32)
            self.t1 = A.take([4], I32)
            self.t2 = A.take([4], I32)
            self.fl = A.take([NT, 4], I32)
            self.ef = A.take([4], F32)
            self.negm = A.take([1], F32)
            self.mm = A.take([4], F32)
            self.qq = A.take([4], F32)
            self.yf = A.take([4], F32)
            self.ff = A.take([4], F32)
            self.zt = A.take([E * NJMAX], I32)
            self.end = A.off

        def init(self, cap):
            self.cap = cap
            self.nj = cap // 128
            MS("dve", self.cum, 0.0, ["cum"])
            DMA("sp", self.zt, c_zero, SEM("zt"), (), ["zt"])
            DMA("sp", tokidx.rearrange("(p c) o -> p (c o)", p=128), self.zt, SEM("zt"), ["zt"], ["tokidx"])


        def tile(self, t, lg_ps, lgn, rbB):
            r = self
            TT("dve", r.logit, lg_ps, rbB, ALU.add, [lgn, "rbB"], ["logit"])
            p.c("dve", lambda e: e.max(r.mx, r.logit), ["logit"], ["mx"])
            p.c("dve", lambda e: e.max_index(r.mi, r.mx, r.logit), ["logit", "mx"], ["mi"])
            TS("dve", r.mask, r.logit, r.mx[:, 3:4], None, ALU.is_ge, None, ["logit", "mx"], ["mask"])
            TS("dve", r.negm, r.mx[:, 0:1], -1.0, None, ALU.mult, None, ["mx"], ["negm"])
            ACT(r.ex, r.mx[:, 0:4], AF.Exp, ["mx", "negm"], ["ex"], bias=r.negm)
            p.c("dve", lambda e: e.tensor_reduce(r.sm, r.ex, AX.X, ALU.add), ["ex"], ["sm"])
            p.c("dve", lambda e: e.reciprocal(r.sm, r.sm), ["sm"], ["sm"])
            TS("dve", r.gates[:, t, :], r.ex, r.sm, None, ALU.mult, None, ["ex", "sm"], ["gates"])
            CP("dve", r.ef, r.mi[:, 0:4], ["mi"], ["ef"])
            CP("pool", r.eidx[:, t, :], r.ef, ["ef"], ["eidx"])
            bk = nbank(0, 5)
            MM(pb[bk][:, 0:E], ltriB, r.mask, True, False, ["ltriB", "mask"], [pbn[bk]])
            MM(pb[bk][:, 0:E], onesB, r.cum, False, True, ["onesB", "cum"], [pbn[bk]])
            CP("dve", r.rank, pb[bk][:, 0:E], [pbn[bk]], ["rank"])
            TT("pool", r.cum, r.cum, r.mask, ALU.add, ["cum", "mask"], ["cum"])
            for k in range(4):
                TS("dve", r.oh, iotaF[:, 0, :], r.ef[:, k:k + 1], None, ALU.is_equal, None, ["iotaF", "ef"], ["oh"])
                TT("dve", r.oh, r.oh, r.rank, ALU.mult, ["oh", "rank"], ["oh"])
                p.c("dve", lambda e, k=k: e.tensor_reduce(r.rk[:, k:k + 1], r.oh, AX.X, ALU.add), ["oh"], ["rk"])
            TS("dve", r.qq, r.rk, 128.0, None, ALU.is_ge, None, ["rk"], ["qq"])
            for i in range(2, r.nj):
                STT("dve", r.qq, r.rk, 128.0 * i, r.qq, ALU.is_ge, ALU.add, ["rk", "qq"], ["qq"])
            STT("dve", r.mm, r.qq, -128.0, r.rk, ALU.mult, ALU.add, ["rk", "qq"], ["mm"])
            STT("dve", r.ff, r.mm, float(E * r.nj), r.qq, ALU.mult, ALU.add, ["mm", "qq"], ["ff"])
            STT("dve", r.ff, r.ef, float(r.nj), r.ff, ALU.mult, ALU.add, ["ef", "ff"], ["ff"])
            TS("dve", r.ff, r.ff, float(128 * E * r.nj - 1), None, ALU.min, None, ["ff"], ["ff"])
            CP("dve", r.fl[:, t, :], r.ff, ["ff"], [f"fl{t}"])
            for k in range(4):
                SCATTER(tokidx, tokid[:, t, k:k + 1], r.fl[:, t, k:k + 1], SEM(f"sc{k}"), [f"fl{t}", "tokid", "tokidx"], [f"tokidx_{t}_{k}"])

    def phase_D(l, CAP):
        pre = f"l{l}_"
        NJ = CAP // 128
        NSL = 10
        NSTG = 6
        LAG = 3
        bgT = A.take([16, E], F32)
        bu1 = A.take([8, E], F32)
        mark = A.off
        bgrow = A.take([2 * D], F32, parts=E)
        DMA("sp", bgrow, W[pre + "b_gu"], SEM("bg"), (), ["bgrow"])
        for c in range(16):
            bk = nbank(0, 6)
            TR(pb[bk][:, 0:E], bgrow[:, c * 128:(c + 1) * 128], identF[0:E, 0:E], ["bgrow", "identF"], [pbn[bk]])
            CP("dve", bgT[:, c, :], pb[bk][:, 0:E], [pbn[bk]], ["bgT"])
        TS("dve", bu1, bgT[:, 8:16, :], 1.0, None, ALU.add, None, ["bgT"], ["bu1"])
        p.barrier()
        A.reset(mark)
        slots = [A.take([4, 1024], BF16) for _ in range(NSL)]
        stg = [A.take([1024], F32) for _ in range(NSTG)]
        idx = A.take([E * NJ], I32)
        DMA("sp", idx, tokidx[0:128 * E * NJ, :].rearrange("(p c) o -> p (c o)", p=128), SEM("idx"), (), ["idx"])
        tokx = A.take([E * NJ], I32)
        TS("dve", tokx, idx, 2, None, ALU.arith_shift_right, None, ["idx"], ["tokx"])
        xg = A.take([NJ, D], BF16)
        xgT = A.take([8, CAP], BF16)
        actT = A.take([8, CAP], BF16)
        gt = [A.take([CAP], F32) for _ in range(2)]
        st = [A.take([CAP], F32) for _ in range(2)]
        ut = [A.take([CAP], F32) for _ in range(2)]
        yst = [A.take([D], F32) for _ in range(2)]
        wgu_v = W[pre + "w_gu"]
        wdn_v = W[pre + "w_dn"]
        ucnt = [0]
        scnt = [0]
        fifo = []
        fcnt = [0]
        ycnt = [0]
        tcnt = [0]

        def open_units(n):
            r = []
            for _ in range(n):
                k = ucnt[0] % NSL
                ucnt[0] += 1
                r.append((slots[k], f"ws{k}"))
            return r

        def gu_chunks(e, units):
            ch = []
            for part in range(2):
                for kh in range(2):
                    wt, wn = units[part * 2 + kh]
                    for kc in range(4):
                        r0 = (kh * 4 + kc) * 128
                        ch.append((wt[:, kc, :], wn, wgu_v[e, r0:r0 + 128, part * 1024:(part + 1) * 1024]))
            return ch

        def dn_chunks(e, units):
            ch = []
            for kh in range(2):
                wt, wn = units[kh]
                for kc in range(4):
                    r0 = (kh * 4 + kc) * 128
                    ch.append((wt[:, kc, :], wn, wdn_v[e, r0:r0 + 128, :]))
            return ch

        def emit_cast():
            dst, wn, i = fifo.pop(0)
            CP("act", dst, stg[i], [f"stg{i}"], [wn])

        def emit_load(chunk):
            dst, wn, src = chunk
            i = scnt[0] % NSTG
            scnt[0] += 1
            DMA("sp", stg[i], src, SEM(f"stg{i}"), (), [f"stg{i}"])
            fifo.append((dst, wn, i))
            if len(fifo) > LAG:
                emit_cast()

        def gather_expert(e):
            for j in range(NJ):
                GATHER(xg[:, j, :], x1b, tokx[:, e * NJ + j:e * NJ + j + 1], SEM("xg"), ["tokx"], ["xg"])

        gu_units = open_units(4)
        dn_units = open_units(2)
        for ch in gu_chunks(0, gu_units) + dn_chunks(0, dn_units):
            emit_load(ch)
        while fifo:
            emit_cast()
        gather_expert(0)
        for e in range(E):
            nxt_gu_units = open_units(4) if e + 1 < E else None
            nxt_gu = gu_chunks(e + 1, nxt_gu_units) if e + 1 < E else []
            if e + 1 == E:
                while fifo:
                    emit_cast()
            for j in range(NJ):
                for half in range(2):
                    bk = 4 + tcnt[0] % 4
                    tcnt[0] += 1
                    pv = pb[bk].bitcast(BF16)
                    for q in range(4):
                        kc = half * 4 + q
                        TR(pv[:, q * 128:(q + 1) * 128], xg[:, j, kc * 128:(kc + 1) * 128], identB,
                           ["xg", "identB"], [pbn[bk]])
                    CP("act" if half == 0 else "dve", xgT[:, half * 4:half * 4 + 4, j * 128:(j + 1) * 128],
                       pv[:, 0:512].rearrange("p (q c) -> p q c", q=4), [pbn[bk]], ["xgT"])
            if e + 1 < E:
                gather_expert(e + 1)
            for fc in range(8):
                f = fcnt[0] % 2
                fcnt[0] += 1
                res = []
                for part in range(2):
                    b0 = nbank(0, 6)
                    b1 = nbank(0, 6)
                    for kc in range(8):
                        wt, wn = gu_units[part * 2 + kc // 4]
                        lhs = wt[:, kc % 4, fc * 128:(fc + 1) * 128]
                        MM(pb[b0], lhs, xgT[:, kc, 0:512], kc == 0, kc == 7, [wn, "xgT"], [pbn[b0]])
                        MM(pb[b1][:, 0:CAP - 512], lhs, xgT[:, kc, 512:CAP], kc == 0, kc == 7, [wn, "xgT"], [pbn[b1]])
                    res.append((b0, b1))
                (g0, g1), (u0, u1) = res
                gn, sn_, un = f"gt{f}", f"st{f}", f"ut{f}"
                for (bk, lo, hi) in ((g0, 0, 512), (g1, 512, CAP)):
                    TS("dve", gt[f][:, lo:hi], pb[bk][:, 0:hi - lo], bgT[:, fc, e:e + 1], LIMIT, ALU.add, ALU.min,
                       [pbn[bk], "bgT"], [gn])
                ACT(st[f], gt[f], AF.Sigmoid, [gn], [sn_], scale=SW_ALPHA)
                for (bk, lo, hi) in ((u0, 0, 512), (u1, 512, CAP)):
                    ACT(ut[f][:, lo:hi], pb[bk][:, 0:hi - lo], AF.Identity, [pbn[bk], "bu1"], [un], bias=bu1[:, fc, e:e + 1])
                TS("dve", ut[f], ut[f], LIMIT + 1.0, 1.0 - LIMIT, ALU.min, ALU.max, [un], [un])
                TT("dve", gt[f], gt[f], st[f], ALU.mult, [gn, sn_], [gn])
                TT("dve", actT[:, fc, :], gt[f], ut[f], ALU.mult, [gn, un], ["actT"])
                for ch in nxt_gu[2 * fc:2 * fc + 2]:
                    emit_load(ch)
            nxt_dn_units = open_units(2) if e + 1 < E else None
            nxt_dn = dn_chunks(e + 1, nxt_dn_units) if e + 1 < E else []
            for j in range(NJ):
                y = ycnt[0] % 2
                ycnt[0] += 1
                ynm = f"yst{y}"
                for half in range(2):
                    bk = nbank(0, 6)
                    for fc in range(8):
                        wt, wn = dn_units[fc // 4]
                        MM(pb[bk], actT[:, fc, j * 128:(j + 1) * 128], wt[:, fc % 4, half * 512:(half + 1) * 512],
                           fc == 0, fc == 7, ["actT", wn], [pbn[bk]])
                    CP("act", yst[y][:, half * 512:(half + 1) * 512], pb[bk], [pbn[bk]], [ynm])
                SCATTER(Ybuf, yst[y], idx[:, e * NJ + j:e * NJ + j + 1], SEM(f"ysc{y}"), [ynm, "idx"], ())
                for ch in nxt_dn[j * 8 // NJ:(j + 1) * 8 // NJ]:
                    emit_load(ch)
            gu_units, dn_units = nxt_gu_units, nxt_dn_units
        assert not fifo
        p.barrier()

    def phase_E(l, route, dst):
        pre = f"l{l}_"
        s_w = SEM("wE")
        gB = A.take([D], F32)
        bB = A.take([D], F32)
        DMA("sp", gB, W[pre + "ln2_g"].partition_broadcast(128), s_w, (), ["gB"])
        DMA("sp", bB, W[pre + "ln2_b"].partition_broadcast(128), s_w, (), ["bB"])
        bdn = A.take([D], F32, parts=E)
        DMA("sp", bdn, W[pre + "b_dn"], s_w, (), ["bdn"])
        ykt = [A.take([4, D], F32) for _ in range(2)]
        yk = [[ykt[s_][:, k_, :] for k_ in range(4)] for s_ in range(2)]
        xin = [A.take([D], F32) for _ in range(2)]
        acc = [A.take([D], F32) for _ in range(2)]
        outt = [A.take([D], F32) for _ in range(2)]
        G = A.take([E], F32)
        oh = A.take([E], F32)
        GT = A.take([128], F32, parts=E)
        stats = A.take([2, 6], F32)
        mv = A.take([2], F32)
        rstd = A.take([1], F32)
        def stage1(t):
            s = t % 2
            DMA("sp", xin[s], x1f[t * 128:(t + 1) * 128, :], SEM(f"Exin{s}"), (), [f"xin{s}"])
            DMA("sp", ykt[s], Ybuf[t * 512:(t + 1) * 512, :].rearrange("(p k) d -> p k d", k=4), SEM(f"yk{s}"), (),
                [f"yk{s}_{k}" for k in range(4)])
            for k in range(4):
                TS("dve", oh, iotaF[:, 0, :], route.eidx[:, t, k:k + 1], route.gates[:, t, k:k + 1],
                   ALU.is_equal, ALU.mult, ["iotaF", "eidx", "gates"], ["oh"])
                if k == 0:
                    CP("dve", G, oh, ["oh"], ["G"])
                else:
                    TT("dve", G, G, oh, ALU.add, ["G", "oh"], ["G"])
            bk = nbank()
            TR(pb[bk][0:E, 0:128], G, identF, ["G", "identF"], [pbn[bk]])
            CP("act", GT, pb[bk][0:E, 0:128], [pbn[bk]], ["GT"])
            bks = [nbank(), nbank()]
            for half in range(2):
                MM(pb[bks[half]], GT, bdn[:, half * 512:(half + 1) * 512], True, True, ["GT", "bdn"], [pbn[bks[half]]])
            return bks

        def stage2(t, bks):
            s = t % 2
            an = f"acc{s}"
            for half in range(2):
                STT("dve", acc[s][:, half * 512:(half + 1) * 512], xin[s][:, half * 512:(half + 1) * 512], DN_ALPHA,
                    pb[bks[half]], ALU.mult, ALU.add, [f"xin{s}", pbn[bks[half]]], [an])
            for k in range(4):
                STT("dve", acc[s], yk[s][k], route.gates[:, t, k:k + 1], acc[s], ALU.mult, ALU.add,
                    [f"yk{s}_{k}", "gates", an], [an])
            layer_norm_tile(acc[s], an, outt[s], f"outt{s}", gB, bB, stats, mv, rstd)
            DMA("sp", dst[t * 128:(t + 1) * 128, :], outt[s], SEM(f"Eout{s}"), [f"outt{s}"], ())

        nb_ = stage1(0)
        for t in range(NT):
            cur = nb_
            if t + 1 < NT:
                nb_ = stage1(t + 1)
            stage2(t, cur)
        p.barrier()

    A.reset(KEEP)
    route = Route()
    KEEP2 = route.end
    def finish():
        p.emit()
        return nc, p, A

    phase_A0(x_in)
    if stop_after == "A0":
        return finish()
    A.reset(KEEP2)
    attnT = A.take([4, S], BF16)
    mark = A.off
    phase_B0(attnT)
    p.barrier()
    if "attnD" in dbg:
        for h in range(4):
            DMA("sp", attnD[h], attnT[:, h, :], SEM("dbg"), ["attnT"], ())
    if stop_after == "B0":
        return finish()
    A.reset(mark)
    route.init(CAPS[0])
    phase_C(0, attnT, 4, convT, x_in, route)
    if stop_after == "C0":
        return finish()
    A.reset(KEEP2)
    phase_D(0, CAPS[0])
    A.reset(KEEP2)
    phase_E(0, route, x2f if stop_after != "E0" else out_ap)
    if stop_after == "E0":
        return finish()
    phase_A1(x2f)
    if stop_after == "A1":
        return finish()
    A.reset(KEEP2)
    attnT1 = A.take([8, S], BF16)
    mark = A.off
    phase_B1(attnT1)
    p.barrier()
    if "attnD" in dbg:
        for c in range(8):
            DMA("sp", attnD[c], attnT1[:, c, :], SEM("dbg"), ["attnT"], ())
    if stop_after == "B1":
        return finish()
    A.reset(mark)
    route.init(CAPS[1])
    phase_C(1, attnT1, 8, None, x2f, route)
    A.reset(KEEP2)
    phase_D(1, CAPS[1])
    A.reset(KEEP2)
    phase_E(1, route, out_ap)
    return finish()


def make_consts():
    c = {}
    c["c_ident"] = np.eye(128, dtype=np.float32)
    tp = np.arange(128)
    c["c_ltri"] = (tp[:, None] < tp[None, :]).astype(np.float32)
    q = np.arange(512)
    m = np.zeros((128, 4, 512), np.float32)
    for r in range(4):
        m[:, r, :] = ((128 * r + tp[:, None]) <= q[None, :]).astype(np.float32)
    c["c_mask"] = m
    pos = np.arange(S, dtype=np.float32)

    def tables(dim):
        inv = (1.0 / (np.float32(10000.0) ** (np.arange(0, dim, 2, dtype=np.float32) / np.float32(dim)))).astype(np.float32)
        ang = (pos[None, :] * inv[:, None]).astype(np.float32)
        cs = np.cos(ang).astype(np.float32)
        sn = np.sin(ang).astype(np.float32)
        return np.concatenate([cs, cs], 0), np.concatenate([-sn, sn], 0)
    c["c_cos64"], c["c_sin64"] = tables(64)
    c["c_cos128"], c["c_sin128"] = tables(128)
    io = np.zeros((128, 3, 32), np.float32)
    io[:, 0, :] = np.arange(32)
    c["c_iota"] = io
    tk = (np.arange(NT)[None, :] * 128 + np.arange(128)[:, None]).astype(np.int32)
    c["c_tokid"] = (4 * tk[:, :, None] + np.arange(4)[None, None, :]).astype(np.int32)
    c["c_zero"] = np.full((128, E * NJMAX), 4 * S, np.int32)
    return {k: np.ascontiguousarray(v) for k, v in c.items()}


_CACHE = {}


def kernel(**inputs):
    if "nc" not in _CACHE:
        _CACHE["nc"] = build_program()[0]
    nc = _CACHE["nc"]
    consts = make_consts()
    x = np.asarray(inputs["x"], dtype=np.float32)
    shared = {k: np.ascontiguousarray(np.asarray(v)) for k, v in inputs.items() if k != "x"}
    in_maps = []
    for b in range(8):
        m = {"x": np.ascontiguousarray(x[b])}
        m.update(shared)
        m.update(consts)
        in_maps.append(m)
    res = run_bass_kernel_spmd(nc, in_maps, core_ids=list(range(8)))
    return np.stack([np.asarray(r["out"]) for r in res.results], 0).astype(np.float32)
```
